# Optimizing a Trainium2 kernel written in Bass

```python
import math
import jax
import jax.numpy as jnp
from jax import lax
import numpy as np

D_MODEL = 1024
BATCH = 8
SEQ = 4096
DEPTH = 2

HG_HEADS = 8
HG_DK = D_MODEL // HG_HEADS
HG_CHUNK = 64
AT_HEADS = 16
AT_DH = 64
Q_LORA = 384
KV_LORA = 256
IDX_HEADS = 8
IDX_DIM = 64
TOPK_MAX = 256
QBLK = 128
REL_BUCKETS = 32
REL_MAX_DIST = 128
D_FF = 2816
CONV_W = 3
PLE_DIM = 256
EPS = 1e-6
N_A = (DEPTH + 1) // 2
N_B = DEPTH // 2

kernel_name = 'hybrid_hgrn2_dsa_convffn'


def rms_norm(x, g):
    xf = x.astype(jnp.float32)
    y = xf * lax.rsqrt(jnp.mean(xf * xf, axis=-1, keepdims=True) + EPS)
    return (y * g.astype(jnp.float32)).astype(x.dtype)


def rel_bucket(n):
    max_exact = REL_BUCKETS // 2
    nf = jnp.maximum(n, 1).astype(jnp.float32)
    large = max_exact + (jnp.log(nf / max_exact) / math.log(REL_MAX_DIST / max_exact)
                         * (REL_BUCKETS - max_exact)).astype(jnp.int32)
    large = jnp.minimum(large, REL_BUCKETS - 1)
    return jnp.where(n < max_exact, n, large)


def hgrn2_mixer(xn, w_in, lb, onorm, w_out):
    B, L, _ = xn.shape
    proj = xn @ w_in
    q, f, v, g = jnp.split(proj, 4, axis=-1)
    q = jax.nn.silu(q.astype(jnp.float32))
    f = lb + (1.0 - lb) * jax.nn.sigmoid(f.astype(jnp.float32))
    log_f = jnp.log(f)
    k = 1.0 - f
    v = v.astype(jnp.float32)
    nC = L // HG_CHUNK

    def to_chunks(a):
        return a.reshape(B, nC, HG_CHUNK, HG_HEADS, HG_DK).transpose(1, 0, 3, 2, 4)

    tri = jnp.tril(jnp.ones((HG_CHUNK, HG_CHUNK), dtype=bool))

    def step(S, inp):
        qc, kc, vc, lfc = inp
        b = jnp.cumsum(lfc, axis=2)
        diff = b[:, :, :, None, :] - b[:, :, None, :, :]
        decay = jnp.exp(jnp.where(tri[:, :, None], diff, -jnp.inf))
        attn = jnp.einsum('bhtd,bhsd,bhtsd->bhts', qc, kc, decay)
        o = jnp.einsum('bhts,bhsv->bhtv', attn, vc) + jnp.einsum('bhtd,bhdv->bhtv', qc * jnp.exp(b), S)
        b_last = b[:, :, -1, :]
        S = jnp.exp(b_last)[..., None] * S + jnp.einsum(
            'bhsd,bhsv->bhdv', kc * jnp.exp(b_last[:, :, None, :] - b), vc)
        return S, o

    S0 = jnp.zeros((B, HG_HEADS, HG_DK, HG_DK), jnp.float32)
    _, o = lax.scan(step, S0, (to_chunks(q), to_chunks(k), to_chunks(v), to_chunks(log_f)))
    o = o.transpose(1, 0, 3, 2, 4).reshape(B, L, HG_HEADS, HG_DK)
    o = rms_norm(o, onorm.reshape(HG_HEADS, HG_DK)).reshape(B, L, D_MODEL)
    o = o * jax.nn.silu(g.astype(jnp.float32))
    return o.astype(xn.dtype) @ w_out


def dsa_mixer(xn, w_in, q_norm, kv_norm, w_uq, w_uk, w_uv, w_qidx, w_out, rel_bias):
    B, L, _ = xn.shape
    proj = xn @ w_in
    c_q, c_kv, k_idx, w_idx = jnp.split(
        proj, [Q_LORA, Q_LORA + KV_LORA, Q_LORA + KV_LORA + IDX_DIM], axis=-1)
    c_q = rms_norm(c_q, q_norm)
    c_kv = rms_norm(c_kv, kv_norm)
    q_nope = (c_q @ w_uq).reshape(B, L, AT_HEADS, AT_DH)
    q_idx = (c_q @ w_qidx).reshape(B, L, IDX_HEADS, IDX_DIM)
    w_idx = w_idx.astype(jnp.float32) * (IDX_HEADS ** -0.5 * IDX_DIM ** -0.5)
    k_idx = k_idx.astype(jnp.float32)
    topk = max(1, min(TOPK_MAX, L // 4))
    nblk = L // QBLK
    key_pos = jnp.arange(L, dtype=jnp.int32)

    def blocks(a):
        return jnp.moveaxis(a.reshape(B, nblk, QBLK, *a.shape[2:]), 1, 0)

    def attend(inp):
        qn, qi, wi, start = inp
        t = start + jnp.arange(QBLK, dtype=jnp.int32)
        causal = key_pos[None, :] <= t[:, None]
        rel = jax.nn.relu(jnp.einsum('bthd,bsd->bths', qi.astype(jnp.float32), k_idx))
        score = jnp.einsum('bths,bth->bts', rel, wi)
        score = jnp.where(causal[None], score, -jnp.inf)
        _, idx = lax.top_k(score, topk)
        valid = idx <= t[None, :, None]
        c_sel = jax.vmap(lambda c, i: c[i])(c_kv, idx)
        q_lat = jnp.einsum('bthd,hdc->bthc', qn, w_uk)
        logits = jnp.einsum('bthc,btkc->bhtk', q_lat.astype(jnp.float32),
                            c_sel.astype(jnp.float32)) * (AT_DH ** -0.5)
        bucket = rel_bucket(jnp.maximum(t[None, :, None] - idx, 0))
        logits = logits + jnp.moveaxis(rel_bias[bucket].astype(jnp.float32), -1, 1)
        logits = jnp.where(valid[:, None], logits, -jnp.inf)
        probs = jax.nn.softmax(logits, axis=-1).astype(c_sel.dtype)
        o_lat = jnp.einsum('bhtk,btkc->bthc', probs, c_sel)
        return jnp.einsum('bthc,hcd->bthd', o_lat, w_uv)

    starts = jnp.arange(nblk, dtype=jnp.int32) * QBLK
    o = lax.map(attend, (blocks(q_nope), blocks(q_idx), blocks(w_idx), starts))
    o = jnp.moveaxis(o, 0, 1).reshape(B, L, AT_HEADS * AT_DH)
    return o @ w_out


def conv_ffn(xn, w_up, conv_w, conv_b, w_down):
    u = xn @ w_up
    C = u.shape[-1]
    uc = lax.conv_general_dilated(
        u, conv_w.astype(u.dtype).reshape(CONV_W, 1, C), window_strides=(1,),
        padding=[(CONV_W - 1, 0)], dimension_numbers=('NWC', 'WIO', 'NWC'),
        feature_group_count=C) + conv_b.astype(u.dtype)
    gate, val = jnp.split(uc, 2, axis=-1)
    return (jax.nn.silu(gate) * val) @ w_down


def setup_inputs(seed: int = 0) -> dict:
    key = jax.random.key(seed)
    ks = jax.random.split(key, 26)

    def w(k, shape, fan_in):
        return jax.random.normal(k, shape, jnp.float32) * fan_in ** -0.5

    def g(k, shape):
        return 1.0 + 0.05 * jax.random.normal(k, shape, jnp.float32)

    at_in = Q_LORA + KV_LORA + IDX_DIM + IDX_HEADS
    return {
        'x': jax.random.normal(ks[0], (BATCH, SEQ, D_MODEL), jnp.float32),
        'p': jax.random.normal(ks[1], (DEPTH, BATCH, SEQ, PLE_DIM), jnp.float32),
        'hg_norm': g(ks[2], (N_A, D_MODEL)),
        'hg_w_in': w(ks[3], (N_A, D_MODEL, 4 * D_MODEL), D_MODEL),
        'hg_lb': 0.1 * jax.random.normal(ks[4], (DEPTH + 1, D_MODEL), jnp.float32),
        'hg_onorm': g(ks[5], (N_A, D_MODEL)),
        'hg_w_out': w(ks[6], (N_A, D_MODEL, D_MODEL), D_MODEL),
        'at_norm': g(ks[7], (N_B, D_MODEL)),
        'at_w_in': w(ks[8], (N_B, D_MODEL, at_in), D_MODEL),
        'at_q_norm': g(ks[9], (N_B, Q_LORA)),
        'at_kv_norm': g(ks[10], (N_B, KV_LORA)),
        'at_w_uq': w(ks[11], (N_B, Q_LORA, AT_HEADS * AT_DH), Q_LORA),
        'at_w_uk': w(ks[12], (N_B, AT_HEADS, AT_DH, KV_LORA), KV_LORA),
        'at_w_uv': w(ks[13], (N_B, AT_HEADS, KV_LORA, AT_DH), KV_LORA),
        'at_w_qidx': w(ks[14], (N_B, Q_LORA, IDX_HEADS * IDX_DIM), Q_LORA),
        'at_w_out': w(ks[15], (N_B, AT_HEADS * AT_DH, D_MODEL), AT_HEADS * AT_DH),
        'rel_bias': 0.5 * jax.random.normal(ks[16], (REL_BUCKETS, AT_HEADS), jnp.float32),
        'ff_norm': g(ks[17], (DEPTH, D_MODEL)),
        'ff_w_up': w(ks[18], (DEPTH, D_MODEL, 2 * D_FF), D_MODEL),
        'ff_conv_w': w(ks[19], (DEPTH, CONV_W, 2 * D_FF), CONV_W),
        'ff_conv_b': 0.02 * jax.random.normal(ks[20], (DEPTH, 2 * D_FF), jnp.float32),
        'ff_w_down': w(ks[21], (DEPTH, D_FF, D_MODEL), D_FF),
        'ple_norm': g(ks[22], (DEPTH, D_MODEL)),
        'ple_w_gate': w(ks[23], (DEPTH, D_MODEL, D_MODEL), D_MODEL),
        'ple_w_proj': w(ks[24], (DEPTH, PLE_DIM, D_MODEL), PLE_DIM),
        'final_norm': g(ks[25], (D_MODEL,)),
    }


def reference(x, p, hg_norm, hg_w_in, hg_lb, hg_onorm, hg_w_out,
              at_norm, at_w_in, at_q_norm, at_kv_norm, at_w_uq, at_w_uk, at_w_uv,
              at_w_qidx, at_w_out, rel_bias,
              ff_norm, ff_w_up, ff_conv_w, ff_conv_b, ff_w_down,
              ple_norm, ple_w_gate, ple_w_proj, final_norm):
    lb_all = jnp.cumsum(jax.nn.softmax(hg_lb.astype(jnp.float32), axis=0), axis=0)
    h = x
    for i in range(DEPTH):
        j = i // 2
        if i % 2 == 0:
            h = h + hgrn2_mixer(rms_norm(h, hg_norm[j]), hg_w_in[j], lb_all[i],
                                hg_onorm[j], hg_w_out[j]).astype(h.dtype)
        else:
            h = h + dsa_mixer(rms_norm(h, at_norm[j]), at_w_in[j], at_q_norm[j], at_kv_norm[j],
                              at_w_uq[j], at_w_uk[j], at_w_uv[j], at_w_qidx[j], at_w_out[j],
                              rel_bias).astype(h.dtype)
        h = h + conv_ffn(rms_norm(h, ff_norm[i]), ff_w_up[i], ff_conv_w[i], ff_conv_b[i], ff_w_down[i])
        gate = jax.nn.sigmoid(rms_norm(h, ple_norm[i]) @ ple_w_gate[i])
        h = h + gate * (p[i] @ ple_w_proj[i])
    return rms_norm(h, final_norm)
```

```python
import math
from contextlib import ExitStack

import numpy as np
import ml_dtypes
import concourse.bass as bass
import concourse.mybir as mybir
from concourse.bass_utils import run_bass_kernel_spmd

F32 = mybir.dt.float32
BF16 = mybir.dt.bfloat16
AF = mybir.ActivationFunctionType
ALU = mybir.AluOpType
AX = mybir.AxisListType

D = 1024
TT = 512
NSUB = 4
DFF = 2816
EPS = 1e-6
NIT = 22
TOPK = 256
NEG = -1.0e30


class Sem:
    def __init__(self, h):
        self.h = h
        self.cnt = 0


class Reg:
    __slots__ = ("name", "w", "rs")

    def __init__(self, name=""):
        self.name = name
        self.w = None
        self.rs = {}


class Buf:
    def __init__(self, t, name, nreg=1):
        self.t = t
        self.reg = Reg(name)
        self.regs = [Reg(f"{name}{i}") for i in range(nreg)] if nreg > 1 else [self.reg]

    def __getitem__(self, k):
        return self.t[k]


def _regs(lst):
    out = []
    for b in lst:
        if isinstance(b, Reg):
            out.append(b)
        elif isinstance(b, Buf):
            out.append(b.reg)
        else:
            raise TypeError(type(b))
    return out


class Eng:
    def __init__(self, e, name, sem, same=True):
        self.e = e
        self.name = name
        self.sem = sem
        self.seen = {}
        self.same = same

    def wait(self, tok):
        if tok is None:
            return
        sem, val = tok
        if sem is self.sem and not self.same:
            return
        if self.seen.get(id(sem), 0) >= val:
            return
        self.e.wait_ge(sem.h, val)
        self.seen[id(sem)] = val


class KB:
    def __init__(self, nc, es):
        self.nc = nc
        self.es = es
        mk = lambda n: Sem(es.enter_context(nc.semaphore(n)))
        self.PE = Eng(nc.tensor, "pe", mk("s_pe"), same=False)
        self.ACT = Eng(nc.scalar, "act", mk("s_act"))
        self.DVE = Eng(nc.vector, "dve", mk("s_dve"))
        self.POOL = Eng(nc.gpsimd, "pool", mk("s_pool"))
        self.SP = Eng(nc.sync, "sp", mk("s_sp"))
        self.compute = [self.PE, self.ACT, self.DVE, self.POOL]
        self.nsem = 5
        self.ninst = 0

    def dsem(self, name):
        self.nsem += 1
        return Sem(self.es.enter_context(self.nc.semaphore(name)))

    def _pre(self, E, r, w):
        for b in r:
            E.wait(b.w)
        for b in w:
            E.wait(b.w)
            for t in b.rs.values():
                E.wait(t)

    def _post(self, tok, r, w):
        for b in r:
            b.rs[id(tok[0])] = tok
        for b in w:
            b.w = tok
            b.rs = {}

    SEM_LIMIT = 10 ** 9

    def op(self, E, emit, r=(), w=(), sig=True):
        r = _regs(r)
        w = _regs(w)
        if E.sem.cnt >= self.SEM_LIMIT:
            self.nsem += 1
            E.sem = Sem(self.es.enter_context(self.nc.semaphore(f"s_{E.name}_{self.nsem}")))
        self._pre(E, r, w)
        ins = emit()
        self.ninst += 1
        if sig:
            E.sem.cnt += 1
            ins.then_inc(E.sem.h, 1)
            tok = (E.sem, E.sem.cnt)
        else:
            tok = (E.sem, E.sem.cnt + 1)
        self._post(tok, r, w)
        return tok

    def dma(self, out, in_, sem, r=(), w=(), Q=None):
        Q = Q or self.SP
        r = _regs(r)
        w = _regs(w)
        self._pre(Q, r, w)
        sem.cnt += 16
        Q.e.dma_start(out=out, in_=in_).then_inc(sem.h, 16)
        self.ninst += 1
        tok = (sem, sem.cnt)
        self._post(tok, r, w)
        return tok

    def barrier(self):
        for E in self.compute + [self.SP]:
            for Fg in self.compute:
                if Fg is not E:
                    E.wait((Fg.sem, Fg.sem.cnt))

    def mm(self, out, lhsT, rhs, start=True, stop=True, r=(), w=(), sig=None):
        if sig is None:
            sig = stop
        return self.op(self.PE, lambda: self.nc.tensor.matmul(out, lhsT, rhs, start=start, stop=stop),
                       r, w, sig)

    def tr(self, out, in_, ident, r=(), w=(), sig=True):
        return self.op(self.PE, lambda: self.nc.tensor.transpose(out, in_, ident), r, w, sig)

    def act(self, out, in_, func, r=(), w=(), bias=None, scale=None, accum=None):
        kw = {}
        if bias is not None:
            kw["bias"] = bias
        if scale is not None:
            kw["scale"] = scale
        if accum is not None:
            kw["accum_out"] = accum
        return self.op(self.ACT, lambda: self.nc.scalar.activation(out=out, in_=in_, func=func, **kw), r, w)

    def ts(self, E, out, in0, s1, s2, op0, op1=None, r=(), w=(), accum=None):
        kw = {}
        if op1 is not None:
            kw["op1"] = op1
        if accum is not None:
            kw["accum_out"] = accum
        return self.op(E, lambda: E.e.tensor_scalar(out=out, in0=in0, scalar1=s1, scalar2=s2, op0=op0, **kw), r, w)

    def tt(self, E, out, in0, in1, op, r=(), w=()):
        return self.op(E, lambda: E.e.tensor_tensor(out=out, in0=in0, in1=in1, op=op), r, w)

    def stt(self, E, out, in0, scalar, in1, op0, op1, r=(), w=()):
        return self.op(E, lambda: E.e.scalar_tensor_tensor(out=out, in0=in0, scalar=scalar, in1=in1,
                                                           op0=op0, op1=op1), r, w)

    def copy(self, E, out, in_, r=(), w=()):
        if E is self.ACT:
            return self.act(out, in_, AF.Copy, r, w)
        return self.op(E, lambda: E.e.tensor_copy(out=out, in_=in_), r, w)

    def memset(self, E, ap, val, w=()):
        return self.op(E, lambda: E.e.memset(ap, val), (), w)


def rel_bucket_np(n):
    n = np.asarray(n)
    nf = np.maximum(n, 1).astype(np.float32)
    large = 16 + (np.log(nf / np.float32(16)) / np.float32(math.log(8.0)) * np.float32(16)).astype(np.int32)
    large = np.minimum(large, 31)
    return np.where(n < 16, n, large)


def host_consts():
    c = {}
    c["c_ident"] = np.eye(128, dtype=np.float32).astype(ml_dtypes.bfloat16)
    s = np.arange(128)
    c["c_cmaskT"] = (s[:, None] <= s[None, :]).astype(np.float32)
    c["c_negmask"] = np.where(s[None, :] <= s[:, None], 0.0, NEG).astype(np.float32)
    rm = np.ones((128, TT), np.float32)
    rm[:, ::128] = 0.0
    c["c_reset"] = rm
    n = np.arange(-127, 257)
    b = rel_bucket_np(np.maximum(n, 0))
    oh = np.zeros((32, 384), np.float32)
    oh[b, np.arange(384)] = 1.0
    oh[31, :] -= 1.0
    c["c_oh"] = np.ascontiguousarray(oh[:, ::-1])
    c["c_pow2"] = np.tile((0.5 ** np.arange(1, NIT + 1)).astype(np.float32)[None, :], (128, 1))
    return c


CONST_SPECS = [("c_ident", [128, 128], BF16), ("c_cmaskT", [128, 128], F32), ("c_negmask", [128, 128], F32),
               ("c_reset", [128, TT], F32), ("c_oh", [32, 384], F32), ("c_pow2", [128, NIT], F32)]

IN_SPECS = [
    ("hg_norm", [1, 1024]), ("hg_w_in", [1, 1024, 4096]), ("hg_lb", [3, 1024]), ("hg_onorm", [1, 1024]),
    ("hg_w_out", [1, 1024, 1024]), ("at_norm", [1, 1024]), ("at_w_in", [1, 1024, 712]),
    ("at_q_norm", [1, 384]), ("at_kv_norm", [1, 256]), ("at_w_uq", [1, 384, 1024]),
    ("at_w_uk", [1, 16, 64, 256]), ("at_w_uv", [1, 16, 256, 64]), ("at_w_qidx", [1, 384, 512]),
    ("at_w_out", [1, 1024, 1024]), ("rel_bias", [32, 16]), ("ff_norm", [2, 1024]),
    ("ff_w_up", [2, 1024, 5632]), ("ff_conv_w", [2, 3, 5632]), ("ff_conv_b", [2, 5632]),
    ("ff_w_down", [2, 2816, 1024]), ("ple_norm", [2, 1024]), ("ple_w_gate", [2, 1024, 1024]),
    ("ple_w_proj", [2, 256, 1024]), ("final_norm", [1024]),
]


def build(L, stop_after=None):
    NT = L // TT
    NBLK = L // 128
    nc = bass.Bass("TRN2", target_bir_lowering=False)
    dr = {}
    dr["x"] = nc.dram_tensor("x", [L, D], F32, kind="ExternalInput").ap()
    dr["p"] = nc.dram_tensor("p", [2, L, 256], F32, kind="ExternalInput").ap()
    for n, shp in IN_SPECS:
        dr[n] = nc.dram_tensor(n, shp, F32, kind="ExternalInput").ap()
    for n, shp, dt in CONST_SPECS:
        dr[n] = nc.dram_tensor(n, shp, dt, kind="ExternalInput").ap()
    y = nc.dram_tensor("y", [L, D], F32, kind="ExternalOutput").ap()

    WS = {}

    def wspec(name, src, K, gain, panels, scale=1.0):
        KC = K // 128
        NP = max(sum(n for _, n in segs) for segs in panels)
        scr = nc.dram_tensor("scr_" + name, [len(panels), 128, KC, NP], BF16, kind="Internal").ap()
        WS[name] = dict(src=src, KC=KC, gain=gain, panels=panels, scale=scale, scr=scr, NP=NP)

    colp = lambda N, w=512: [[(c, min(w, N - c))] for c in range(0, N, w)]
    wspec("hg_in", dr["hg_w_in"][0], 1024, ("hg_norm", 0), colp(4096))
    wspec("hg_out", dr["hg_w_out"][0], 1024, ("hg_onorm", 0), colp(1024))
    for l in range(2):
        wspec(f"ff_up{l}", dr["ff_w_up"][l], 1024, ("ff_norm", l),
              [[(256 * j, 256), (DFF + 256 * j, 256)] for j in range(11)])
        wspec(f"ff_dn{l}", dr["ff_w_down"][l], 2816, None, colp(1024))
        wspec(f"ple_g{l}", dr["ple_w_gate"][l], 1024, ("ple_norm", l), colp(1024))
        wspec(f"ple_p{l}", dr["ple_w_proj"][l], 256, None, colp(1024))
    wspec("at_in", dr["at_w_in"][0], 1024, ("at_norm", 0), [[(0, 384), (640, 72)], [(384, 256)]])
    wspec("at_uq", dr["at_w_uq"][0], 384, ("at_q_norm", 0), colp(1024))
    wspec("at_qi", dr["at_w_qidx"][0], 384, ("at_q_norm", 0), colp(512))
    wspec("at_out", dr["at_w_out"][0], 1024, None, colp(1024))
    rbv = nc.dram_tensor("scr_rbv", [16, 384], F32, kind="Internal").ap()

    with ExitStack() as es:
        kb = KB(nc, es)
        PE, ACT, DVE, POOL, SP = kb.PE, kb.ACT, kb.DVE, kb.POOL, kb.SP

        uid = {"n": 0}

        def sb(name, shape, dt, nreg=1, st=es):
            uid["n"] += 1
            return Buf(st.enter_context(nc.sbuf_tensor(f"{name}_{uid['n']}", shape, dt)), name, nreg)

        def psb(name, shape, dt):
            return Buf(es.enter_context(nc.psum_tensor(name, shape, dt)), name)

        PB = [psb(f"pb{i}", [128, 512], F32) for i in range(6)]
        PT = [psb(f"pt{i}", [128, 1024], BF16) for i in range(2)]
        rot = {"b": 0, "t": 0}

        def bank(lst=None):
            lst = lst or PB
            rot["b"] += 1
            return lst[rot["b"] % len(lst)]

        def tbank():
            rot["t"] += 1
            return PT[rot["t"] % 2]

        ident = sb("ident", [128, 128], BF16)
        cmaskT = sb("cmaskT", [128, 128], F32)
        negmask = sb("negmask", [128, 128], F32)
        pow2 = sb("pow2", [128, NIT], F32)
        ones_bf = sb("ones_bf", [128, 128], BF16)
        eps_t = sb("eps_t", [128, 1], F32)
        lb = sb("lb", [128, 8], F32)
        oml = sb("oml", [128, 8], F32)
        noml = sb("noml", [128, 8], F32)
        cw = sb("cw", [128, 2, 3, 44], F32)
        cb = sb("cb", [128, 2, 44], F32)
        carry = sb("carry", [128, 2, 44, 2], F32)
        S = sb("S", [128, 8, 128], F32, nreg=8)
        ckvT = sb("ckvT", [128, 2, L], BF16)
        ckv1 = sb("ckv1", [128, NBLK, 258], BF16)
        kidxT = sb("kidxT", [128, L], BF16)
        biasT = sb("biasT", [128, 2, 16, 128], F32)
        wuk = sb("wuk", [128, 8, 256], BF16)
        wuv = sb("wuv", [128, 2, 16, 64], BF16)
        h = sb("h", [128, NSUB, D], F32, nreg=NSUB)
        xnT = sb("xnT", [128, 8, TT], BF16)
        xs = [sb("xs0", [128, D], BF16)] * 2
        junk = xs[0]
        gfin = sb("gfin", [128, 1024], F32)
        ss = sb("ss", [128, NSUB], F32)
        rstd = sb("rstd", [128, NSUB], F32)
        NSLOT = 5
        ring_sem = [kb.dsem(f"s_ring{i}") for i in range(NSLOT)]
        s_x = kb.dsem("s_x")
        s_p = kb.dsem("s_p")
        s_y = kb.dsem("s_y")
        s_rm = kb.dsem("s_rm")

        prep_sems = []

        def psem():
            sm = kb.dsem(f"s_prep{len(prep_sems)}")
            prep_sems.append(sm)
            return sm

        def ld(dst_buf, dst_ap, src_ap, sm=None):
            kb.dma(dst_ap, src_ap, sm or psem(), w=[dst_buf])

        ld(ident, ident[:], dr["c_ident"])
        ld(cmaskT, cmaskT[:], dr["c_cmaskT"])
        ld(negmask, negmask[:], dr["c_negmask"])
        ld(pow2, pow2[:], dr["c_pow2"])
        kb.memset(DVE, ones_bf[:], 1.0, w=[ones_bf])
        kb.memset(DVE, eps_t[:], EPS, w=[eps_t])
        kb.memset(DVE, S[:], 0.0, w=S.regs)
        kb.memset(DVE, carry[:], 0.0, w=[carry])
        kb.memset(POOL, ckv1[:], 1.0, w=[ckv1])

        with ExitStack() as st:
            def gain_tile(name, src1d, KC):
                g = sb("g_" + name, [128, KC], F32, st=st)
                with nc.allow_non_contiguous_dma(reason="tiny gain vector load"):
                    ld(g, g[:], src1d.rearrange("(kc p) -> p kc", p=128))
                return g

            gains = {}
            for gname, l, KC in [("hg_norm", 0, 8), ("hg_onorm", 0, 8), ("ff_norm", 0, 8), ("ff_norm", 1, 8),
                                 ("ple_norm", 0, 8), ("ple_norm", 1, 8), ("at_norm", 0, 8), ("at_q_norm", 0, 3),
                                 ("at_kv_norm", 0, 2)]:
                gains[(gname, l)] = gain_tile(f"{gname}{l}", dr[gname][l], KC)
            lbr = sb("lbr", [128, 3, 8], F32, st=st)
            lbe = sb("lbe", [128, 3, 8], F32, st=st)
            lbs = sb("lbs", [128, 8], F32, st=st)
            with nc.allow_non_contiguous_dma(reason="tiny vector load"):
                ld(lbr, lbr[:], dr["hg_lb"].rearrange("s (kc p) -> p s kc", p=128))
                s_cw, s_cb = psem(), psem()
                for l in range(2):
                    ld(cw, cw[:, l], dr["ff_conv_w"][l].rearrange("t (c p) -> p t c", p=128), s_cw)
                    ld(cb, cb[:, l], dr["ff_conv_b"][l].rearrange("(c p) -> p c", p=128), s_cb)
            kb.act(lbe[:], lbr[:], AF.Exp, r=[lbr], w=[lbe])
            kb.tt(DVE, lbs[:], lbe[:, 0, :], lbe[:, 1, :], ALU.add, r=[lbe], w=[lbs])
            kb.tt(DVE, lbs[:], lbs[:], lbe[:, 2, :], ALU.add, r=[lbe, lbs], w=[lbs])
            kb.op(DVE, lambda: nc.vector.reciprocal(out=lbs[:], in_=lbs[:]), r=[lbs], w=[lbs])
            kb.tt(DVE, lb[:], lbe[:, 0, :], lbs[:], ALU.mult, r=[lbe, lbs], w=[lb])
            kb.ts(DVE, oml[:], lb[:], -1.0, 1.0, ALU.mult, ALU.add, r=[lb], w=[oml])
            kb.ts(DVE, noml[:], oml[:], -1.0, None, ALU.mult, r=[oml], w=[noml])

            ld(gfin, gfin[:], dr["final_norm"].partition_broadcast(128))
            stg = [sb(f"stg{i}", [128, 8, 512], F32, st=st) for i in range(2)]
            stb = [sb(f"stb{i}", [128, 8, 512], BF16, st=st) for i in range(2)]
            s_stg = [kb.dsem(f"s_stg{i}") for i in range(2)]
            s_stb = [kb.dsem(f"s_stb{i}") for i in range(2)]
            cnt = {"i": 0, "e": 0}
            cvt_eng = [DVE, POOL, ACT]
            scr_regs = {}
            for name, wsp in WS.items():
                scr_regs[name] = Reg("scr_" + name)
                KC = wsp["KC"]
                src = wsp["src"].rearrange("(kc p) n -> p kc n", p=128)
                g = gains[wsp["gain"]] if wsp["gain"] else None
                for pi, segs in enumerate(wsp["panels"]):
                    npc = sum(n for _, n in segs)
                    for kg in range(0, KC, 8):
                        kcs = min(8, KC - kg)
                        i = cnt["i"] % 2
                        cnt["i"] += 1
                        off = 0
                        for (c0, n) in segs:
                            kb.dma(stg[i][:, 0:kcs, off:off + n], src[:, kg:kg + kcs, c0:c0 + n], s_stg[i],
                                   w=[stg[i]])
                            off += n
                        if g is None:
                            E = cvt_eng[cnt["e"] % 3]
                            cnt["e"] += 1
                            kb.copy(E, stb[i][:, 0:kcs, 0:npc], stg[i][:, 0:kcs, 0:npc], r=[stg[i]], w=[stb[i]])
                        else:
                            for kc in range(kcs):
                                E = cvt_eng[cnt["e"] % 3]
                                cnt["e"] += 1
                                if E is ACT:
                                    kb.act(stb[i][:, kc, 0:npc], stg[i][:, kc, 0:npc], AF.Copy,
                                           r=[stg[i], g], w=[stb[i]], scale=g[:, kg + kc:kg + kc + 1])
                                else:
                                    kb.ts(E, stb[i][:, kc, 0:npc], stg[i][:, kc, 0:npc],
                                          g[:, kg + kc:kg + kc + 1], None, ALU.mult, r=[stg[i], g], w=[stb[i]])
                        kb.dma(wsp["scr"][pi, :, kg:kg + kcs, 0:npc], stb[i][:, 0:kcs, 0:npc], s_stb[i],
                               r=[stb[i]])

            gkv_b = sb("gkv_b", [128, 256], F32, st=st)
            ld(gkv_b, gkv_b[:], dr["at_kv_norm"][0].partition_broadcast(128))
            wuk_f = sb("wuk_f", [128, 8, 256], F32, st=st)
            s_wuk = psem()
            for par in range(2):
                ld(wuk_f, wuk_f[par * 64:(par + 1) * 64, :, :],
                   dr["at_w_uk"][0].rearrange("(j two) d c -> two d j c", two=2)[par], s_wuk)
            for j in range(8):
                kb.stt(DVE, wuk[:, j, :], wuk_f[:, j, :], 0.125, gkv_b[:], ALU.mult, ALU.mult,
                       r=[wuk_f, gkv_b], w=[wuk])
            wuv_f = sb("wuv_f", [128, 2, 16, 64], F32, st=st)
            gkv = gains[("at_kv_norm", 0)]
            s_wuv = psem()
            for cc in range(2):
                ld(wuv_f, wuv_f[:, cc], dr["at_w_uv"][0].rearrange("h (cc c) d -> cc c h d", cc=2)[cc], s_wuv)
            for cc in range(2):
                kb.ts(DVE, wuv[:, cc], wuv_f[:, cc], gkv[:, cc:cc + 1], None, ALU.mult, r=[wuv_f, gkv], w=[wuv])
            rb_sb = sb("rb_sb", [32, 16], F32, st=st)
            oh_sb = sb("oh_sb", [32, 384], F32, st=st)
            rbv_sb = sb("rbv_sb", [16, 384], F32, st=st)
            ld(rb_sb, rb_sb[:], dr["rel_bias"])
            ld(oh_sb, oh_sb[:], dr["c_oh"])
            kb.mm(PB[0][0:16, 0:384], rb_sb[:], oh_sb[:], r=[rb_sb, oh_sb], w=[PB[0]])
            kb.copy(DVE, rbv_sb[:], PB[0][0:16, 0:384], r=[PB[0]], w=[rbv_sb])
            rbv_reg = Reg("rbv")
            kb.dma(rbv, rbv_sb[:], psem(), r=[rbv_sb], w=[rbv_reg])
            s_bias = psem()
            Hk = sb("Hk", [128, 16, 128], F32, st=st)
            for dl in range(2):
                cst = 129 - 128 * dl
                for hh in range(16):
                    src = bass.AP(rbv.tensor, hh * 384 + cst, [[1, 128], [1, 128]])
                    kb.dma(Hk[:, hh, :], src, s_bias, r=[rbv_reg], w=[Hk])
                for hh in range(16):
                    kb.copy(DVE, biasT[:, dl, hh, :], Hk[:, hh, ::-1], r=[Hk], w=[biasT])
            kb.barrier()
            for E in kb.compute + [SP]:
                for sm in prep_sems + s_stb + s_stg:
                    E.wait((sm, sm.cnt))

        ring = [sb(f"ring{i}", [128, 8, 512], BF16) for i in range(NSLOT)]
        def tile_sched():
            sc = []
            sc += [("hg_in", 4), ("hg_in", 5)]
            for gi in range(2):
                sc += [("hg_in", 0 + gi), ("hg_in", 2 + gi), ("hg_in", 6 + gi)]
            sc += [("hg_out", 0), ("hg_out", 1)]

            def ffn(l):
                s2 = [(f"ff_up{l}", j) for j in range(11)]
                for half in range(2):
                    s2 += [(f"ff_dn{l}", half, kg) for kg in range(3)]
                s2 += [(f"ple_g{l}", 0), (f"ple_p{l}", 0), (f"ple_g{l}", 1), (f"ple_p{l}", 1)]
                return s2
            sc += ffn(0)
            sc += [("at_in", 0), ("at_in", 1), ("at_uq", 0), ("at_uq", 1), ("at_qi", 0)]
            sc += [("at_out", 0), ("at_out", 1)] * NSUB
            sc += ffn(1)
            return sc

        sched = tile_sched() * NT
        WR = {"next_load": 0, "next_use": 0, "free": list(range(NSLOT)), "slot_of": {}}

        def _issue_loads():
            while WR["free"] and WR["next_load"] < len(sched):
                i = WR["next_load"]
                it = sched[i]
                name, pi = it[0], it[1]
                wsp = WS[name]
                sl = WR["free"].pop(0)
                KC = wsp["KC"]
                if len(it) == 3:
                    kg = it[2] * 8
                    kcs = min(8, KC - kg)
                else:
                    kg, kcs = 0, KC
                NP = wsp["NP"]
                kb.dma(ring[sl][:, 0:kcs, 0:NP], wsp["scr"][pi, :, kg:kg + kcs, :], ring_sem[sl],
                       w=[ring[sl]])
                WR["slot_of"][i] = sl
                WR["next_load"] += 1

        def wacq(expect):
            i = WR["next_use"]
            assert tuple(sched[i][:len(expect)]) == tuple(expect), (sched[i], expect)
            _issue_loads()
            assert i in WR["slot_of"], "weight ring exhausted"
            WR["next_use"] += 1
            return i, ring[WR["slot_of"][i]]

        def wrel(i):
            WR["free"].append(WR["slot_of"].pop(i))
            _issue_loads()

        def norm_to_xnT():
            for s in range(NSUB):
                kb.act(junk[:], h[:, s, :], AF.Square, r=[h.regs[s]], w=[junk, ss], accum=ss[:, s:s + 1])
            kb.act(rstd[:], ss[:], AF.Sqrt, r=[ss, eps_t], w=[rstd], scale=1.0 / D, bias=eps_t[:])
            kb.op(DVE, lambda: nc.vector.reciprocal(out=rstd[:], in_=rstd[:]), r=[rstd], w=[rstd])
            for s in range(NSUB):
                x_ = xs[s % 2]
                kb.ts(DVE, x_[:], h[:, s, :], rstd[:, s:s + 1], None, ALU.mult, r=[h.regs[s], rstd], w=[x_])
                tb = tbank()
                for kc in range(8):
                    kb.tr(tb[:, kc * 128:(kc + 1) * 128], x_[:, kc * 128:(kc + 1) * 128], ident[:],
                          r=[x_], w=[tb], sig=(kc == 7))
                kb.copy(ACT, xnT[:, :, s * 128:(s + 1) * 128], tb[:].rearrange("p (k t) -> p k t", k=8),
                        r=[tb], w=[xnT])

        def ffn_ple(l, tl, t0):
            norm_to_xnT()
            with ExitStack() as st:
                mT = sb("mT", [128, 22, TT], BF16, st=st)
                U = [sb(f"U{i}", [128, TT + 2], F32, st=st) for i in range(2)]
                cv = [sb(f"cv{i}", [128, TT], F32, st=st) for i in range(4)]
                sgt = [sb(f"sgt{i}", [128, TT], F32, st=st) for i in range(2)]
                for j in range(11):
                    wi, pan = wacq((f"ff_up{l}", j))
                    for blk in range(4):
                        ch = (2 * j + blk) if blk < 2 else (22 + 2 * j + blk - 2)
                        ps = bank()
                        for kc in range(8):
                            kb.mm(ps[:], pan[:, kc, blk * 128:(blk + 1) * 128], xnT[:, kc, :],
                                  start=(kc == 0), stop=(kc == 7), r=[pan, xnT], w=[ps])
                        u = U[blk % 2]
                        c_ = cv[blk]
                        kb.copy(POOL, u[:, 0:2], carry[:, l, ch, :], r=[carry], w=[u])
                        kb.copy(ACT, u[:, 2:TT + 2], ps[:], r=[ps], w=[u])
                        kb.copy(POOL, carry[:, l, ch, :], u[:, TT:TT + 2], r=[u], w=[carry])
                        kb.act(c_[:], ps[:], AF.Identity, r=[ps, cw, cb], w=[c_],
                               scale=cw[:, l, 2, ch:ch + 1], bias=cb[:, l, ch:ch + 1])
                        kb.stt(DVE, c_[:], u[:, 1:TT + 1], cw[:, l, 1, ch:ch + 1], c_[:], ALU.mult, ALU.add,
                               r=[u, cw, c_], w=[c_])
                        kb.stt(DVE, c_[:], u[:, 0:TT], cw[:, l, 0, ch:ch + 1], c_[:], ALU.mult, ALU.add,
                               r=[u, cw, c_], w=[c_])
                    wrel(wi)
                    for q in range(2):
                        sg_ = sgt[q]
                        kb.act(sg_[:], cv[q][:], AF.Silu, r=[cv[q]], w=[sg_])
                        kb.tt(DVE, mT[:, 2 * j + q, :], sg_[:], cv[2 + q][:], ALU.mult, r=[sg_, cv[2 + q]], w=[mT])
                for half in range(2):
                    pans = [wacq((f"ff_dn{l}", half, kg)) for kg in range(3)]
                    for s in range(NSUB):
                        ps = bank()
                        for kc in range(22):
                            pan = pans[kc // 8][1]
                            kb.mm(ps[:], mT[:, kc, s * 128:(s + 1) * 128], pan[:, kc % 8, :],
                                  start=(kc == 0), stop=(kc == 21), r=[pan, mT], w=[ps])
                        hs = h[:, s, half * 512:(half + 1) * 512]
                        kb.tt(DVE, hs, ps[:], hs, ALU.add, r=[ps, h.regs[s]], w=[h.regs[s]])
                    for wi, _ in pans:
                        wrel(wi)
                kb.barrier()
            norm_to_xnT()
            with ExitStack() as st:
                pt = sb("pt", [128, NSUB, 256], F32, st=st)
                ptb = sb("ptb", [128, NSUB, 256], BF16, st=st)
                pT = sb("pT", [128, 2, TT], BF16, st=st)
                sgm = [sb(f"sgm{i}", [128, 512], F32, st=st) for i in range(2)]
                kb.dma(pt[:], dr["p"][l, t0:t0 + TT, :].rearrange("(s p) d -> p s d", p=128), s_p, w=[pt])
                kb.copy(POOL, ptb[:], pt[:], r=[pt], w=[ptb])
                for s in range(NSUB):
                    tb = tbank()
                    for kc in range(2):
                        kb.tr(tb[:, kc * 128:(kc + 1) * 128], ptb[:, s, kc * 128:(kc + 1) * 128], ident[:],
                              r=[ptb], w=[tb], sig=(kc == 1))
                    kb.copy(ACT, pT[:, :, s * 128:(s + 1) * 128],
                            tb[:, 0:256].rearrange("p (k t) -> p k t", k=2), r=[tb], w=[pT])
                for half in range(2):
                    wg, pang = wacq((f"ple_g{l}", half))
                    wp, panp = wacq((f"ple_p{l}", half))
                    for s in range(NSUB):
                        psg = bank()
                        for kc in range(8):
                            kb.mm(psg[:], xnT[:, kc, s * 128:(s + 1) * 128], pang[:, kc, :],
                                  start=(kc == 0), stop=(kc == 7), r=[pang, xnT], w=[psg])
                        psp = bank()
                        for kc in range(2):
                            kb.mm(psp[:], pT[:, kc, s * 128:(s + 1) * 128], panp[:, kc, :],
                                  start=(kc == 0), stop=(kc == 1), r=[panp, pT], w=[psp])
                        sg_ = sgm[s % 2]
                        kb.act(sg_[:], psg[:], AF.Sigmoid, r=[psg], w=[sg_])
                        kb.tt(DVE, sg_[:], sg_[:], psp[:], ALU.mult, r=[sg_, psp], w=[sg_])
                        hs = h[:, s, half * 512:(half + 1) * 512]
                        kb.tt(POOL, hs, hs, sg_[:], ALU.add, r=[sg_, h.regs[s]], w=[h.regs[s]])
                    wrel(wg)
                    wrel(wp)
                kb.barrier()

        def store_h(t0, final):
            with ExitStack() as st:
                ot = sb("ot", [128, NSUB, D], F32, st=st)
                if final:
                    for s in range(NSUB):
                        kb.act(junk[:], h[:, s, :], AF.Square, r=[h.regs[s]], w=[junk, ss], accum=ss[:, s:s + 1])
                    kb.act(rstd[:], ss[:], AF.Sqrt, r=[ss, eps_t], w=[rstd], scale=1.0 / D, bias=eps_t[:])
                    kb.op(DVE, lambda: nc.vector.reciprocal(out=rstd[:], in_=rstd[:]), r=[rstd], w=[rstd])
                    for s in range(NSUB):
                        kb.ts(DVE, ot[:, s, :], h[:, s, :], rstd[:, s:s + 1], None, ALU.mult,
                              r=[h.regs[s], rstd], w=[ot])
                        kb.tt(POOL, ot[:, s, :], ot[:, s, :], gfin[:], ALU.mult, r=[ot, gfin], w=[ot])
                else:
                    for s in range(NSUB):
                        kb.copy(DVE, ot[:, s, :], h[:, s, :], r=[h.regs[s]], w=[ot])
                yreg = Reg("y")
                kb.dma(y[t0:t0 + TT, :].rearrange("(s p) d -> p s d", p=128), ot[:], s_y, r=[ot], w=[yreg])
                for E in kb.compute + [SP]:
                    E.wait((s_y, s_y.cnt))
                kb.barrier()

        for tl in range(NT):
            t0 = tl * TT
            kb.dma(h[:], dr["x"][t0:t0 + TT, :].rearrange("(s p) d -> p s d", p=128), s_x, w=h.regs)

            norm_to_xnT()
            with ExitStack() as st:
                resetm = sb("resetm", [128, TT], F32, st=st)
                kb.dma(resetm[:], dr["c_reset"], s_rm, w=[resetm])
                v_tok = sb("v_tok", [128, NSUB, D], BF16, nreg=NSUB, st=st)
                OG = sb("OG", [128, 8, TT], BF16, st=st)
                qb_ = sb("qb", [128, 4, TT], BF16, nreg=4, st=st)
                kbb = sb("kbb", [128, 4, TT], BF16, nreg=4, st=st)
                gs = sb("gs", [128, 4, TT], F32, nreg=4, st=st)
                t_qs = sb("t_qs", [128, TT], F32, st=st)
                t_sg = sb("t_sg", [128, TT], F32, st=st)
                t_lf = sb("t_lf", [128, TT], F32, st=st)
                t_kk = sb("t_kk", [128, TT], F32, st=st)
                t_cum = sb("t_cum", [128, TT], F32, st=st)
                t_rel = sb("t_rel", [128, TT], F32, st=st)
                t_eb = sb("t_eb", [128, TT], F32, st=st)
                t_enb = sb("t_enb", [128, TT], F32, st=st)
                emid = sb("emid", [128, 4, 4], F32, nreg=4, st=st)
                elast = sb("elast", [128, 4, 4], F32, nreg=4, st=st)
                elm = sb("elm", [128, 4, 4], F32, nreg=4, st=st)
                dlm = sb("dlm", [128, 4], F32, st=st)
                kbt = [sb(f"kbt{i}", [128, 128], BF16, st=st) for i in range(2)]
                ATm = [sb(f"ATm{i}", [128, 128], BF16, st=st) for i in range(2)]
                Sp = [sb(f"Sp{i}", [128, 128], BF16, st=st) for i in range(2)]
                kvt = [sb(f"kvt{i}", [128, 128], F32, st=st) for i in range(2)]
                osq = sb("osq", [128, TT], BF16, st=st)
                rs_ = sb("rs_", [128, TT], F32, st=st)
                rg_ = sb("rg_", [128, TT], F32, st=st)

                for half in range(2):
                    wi, pan = wacq(("hg_in", 4 + half))
                    for s in range(NSUB):
                        ps = bank()
                        for kc in range(8):
                            kb.mm(ps[:], xnT[:, kc, s * 128:(s + 1) * 128], pan[:, kc, :],
                                  start=(kc == 0), stop=(kc == 7), r=[pan, xnT], w=[ps])
                        kb.copy(ACT, v_tok[:, s, half * 512:(half + 1) * 512], ps[:], r=[ps], w=[v_tok.regs[s]])
                    wrel(wi)
                ii = 0
                for gi in range(2):
                    wq, panq = wacq(("hg_in", 0 + gi))
                    wf, panf = wacq(("hg_in", 2 + gi))
                    wg, pang = wacq(("hg_in", 6 + gi))
                    for hl in range(4):
                        hd = gi * 4 + hl
                        cs_ = slice(hl * 128, (hl + 1) * 128)

                        def proj(pan):
                            ps = bank()
                            for kc in range(8):
                                kb.mm(ps[:], pan[:, kc, cs_], xnT[:, kc, :], start=(kc == 0), stop=(kc == 7),
                                      r=[pan, xnT], w=[ps])
                            return ps
                        qps = proj(panq)
                        kb.act(t_qs[:], qps[:], AF.Silu, r=[qps], w=[t_qs])
                        gps = proj(pang)
                        kb.act(gs[:, hl, :], gps[:], AF.Silu, r=[gps], w=[gs.regs[hl]])
                        fps = proj(panf)
                        kb.act(t_sg[:], fps[:], AF.Sigmoid, r=[fps], w=[t_sg])
                        kb.act(t_lf[:], t_sg[:], AF.Ln, r=[t_sg, oml, lb], w=[t_lf],
                               scale=oml[:, hd:hd + 1], bias=lb[:, hd:hd + 1])
                        kb.ts(DVE, t_kk[:], t_sg[:], noml[:, hd:hd + 1], oml[:, hd:hd + 1], ALU.mult, ALU.add,
                              r=[t_sg, noml, oml], w=[t_kk])
                        kb.op(DVE, lambda: nc.vector.tensor_tensor_scan(
                            out=t_cum[:], data0=resetm[:], data1=t_lf[:], initial=0.0,
                            op0=ALU.mult, op1=ALU.add), r=[resetm, t_lf], w=[t_cum])
                        cum3 = t_cum[:].rearrange("p (c j) -> p c j", j=128)
                        kb.tt(DVE, t_rel[:].rearrange("p (c j) -> p c j", j=128), cum3,
                              cum3[:, :, 63:64].to_broadcast([128, 4, 128]), ALU.subtract, r=[t_cum], w=[t_rel])
                        kb.act(t_eb[:], t_rel[:], AF.Exp, r=[t_rel], w=[t_eb])
                        kb.act(t_enb[:], t_rel[:], AF.Exp, r=[t_rel], w=[t_enb], scale=-1.0)
                        kb.act(emid[:, hl, :], cum3[:, :, 63], AF.Exp, r=[t_cum], w=[emid.regs[hl]])
                        kb.act(elast[:, hl, :], cum3[:, :, 127], AF.Exp, r=[t_cum], w=[elast.regs[hl]])
                        kb.tt(DVE, dlm[:], cum3[:, :, 127], cum3[:, :, 63], ALU.subtract, r=[t_cum], w=[dlm])
                        kb.act(elm[:, hl, :], dlm[:], AF.Exp, r=[dlm], w=[elm.regs[hl]])
                        kb.tt(DVE, qb_[:, hl, :], t_qs[:], t_eb[:], ALU.mult, r=[t_qs, t_eb], w=[qb_.regs[hl]])
                        kb.tt(POOL, kbb[:, hl, :], t_kk[:], t_enb[:], ALU.mult, r=[t_kk, t_enb], w=[kbb.regs[hl]])
                    wrel(wq)
                    wrel(wf)
                    wrel(wg)
                    for c in range(4):
                        cs = slice(c * 128, (c + 1) * 128)
                        for hl in range(4):
                            hd = gi * 4 + hl
                            i2 = ii % 2
                            ii += 1
                            vh = v_tok[:, c, hd * 128:(hd + 1) * 128]
                            tb = tbank()
                            kb.tr(tb[:, 0:128], kbb[:, hl, cs], ident[:], r=[kbb.regs[hl]], w=[tb])
                            kb.copy(ACT, kbt[i2][:], tb[:, 0:128], r=[tb], w=[kbt[i2]])
                            pa = bank(PB[4:6])
                            kb.mm(pa[:, 0:128], kbb[:, hl, cs], qb_[:, hl, cs], r=[kbb.regs[hl], qb_.regs[hl]], w=[pa])
                            kb.tt(DVE, ATm[i2][:], pa[:, 0:128], cmaskT[:], ALU.mult, r=[pa, cmaskT], w=[ATm[i2]])
                            kb.ts(POOL, Sp[i2][:], S[:, hd, :], emid[:, hl, c:c + 1], None, ALU.mult,
                                  r=[S.regs[hd], emid.regs[hl]], w=[Sp[i2]])
                            kb.mm(PB[hl][:, cs], vh, ATm[i2][:], start=True, stop=False,
                                  r=[v_tok.regs[c], ATm[i2]], w=[PB[hl]], sig=False)
                            kb.mm(PB[hl][:, cs], Sp[i2][:], qb_[:, hl, cs], start=False, stop=True,
                                  r=[Sp[i2], qb_.regs[hl]], w=[PB[hl]])
                            pk = bank(PB[4:6])
                            kb.mm(pk[:, 0:128], kbt[i2][:], vh, r=[kbt[i2], v_tok.regs[c]], w=[pk])
                            kb.act(kvt[i2][:], pk[:, 0:128], AF.Copy, r=[pk, elm.regs[hl]], w=[kvt[i2]],
                                   scale=elm[:, hl, c:c + 1])
                            kb.stt(DVE, S[:, hd, :], S[:, hd, :], elast[:, hl, c:c + 1], kvt[i2][:],
                                   ALU.mult, ALU.add, r=[S.regs[hd], elast.regs[hl], kvt[i2]], w=[S.regs[hd]])
                    for hl in range(4):
                        hd = gi * 4 + hl
                        kb.act(osq[:], PB[hl][:], AF.Square, r=[PB[hl]], w=[osq])
                        pa = bank(PB[4:6])
                        kb.mm(pa[:], ones_bf[:], osq[:], r=[ones_bf, osq], w=[pa])
                        kb.act(rs_[:], pa[:], AF.Sqrt, r=[pa, eps_t], w=[rs_], scale=1.0 / 128, bias=eps_t[:])
                        kb.op(DVE, lambda: nc.vector.reciprocal(out=rs_[:], in_=rs_[:]), r=[rs_], w=[rs_])
                        kb.tt(POOL, rg_[:], rs_[:], gs[:, hl, :], ALU.mult, r=[rs_, gs.regs[hl]], w=[rg_])
                        kb.tt(DVE, OG[:, hd, :], PB[hl][:], rg_[:], ALU.mult, r=[PB[hl], rg_], w=[OG])
                for half in range(2):
                    wi, pan = wacq(("hg_out", half))
                    for s in range(NSUB):
                        ps = bank()
                        for j in range(8):
                            kb.mm(ps[:], OG[:, j, s * 128:(s + 1) * 128], pan[:, j, :], start=(j == 0), stop=(j == 7),
                                  r=[OG, pan], w=[ps])
                        hs = h[:, s, half * 512:(half + 1) * 512]
                        kb.tt(DVE, hs, ps[:], hs, ALU.add, r=[ps, h.regs[s]], w=[h.regs[s]])
                    wrel(wi)
                kb.barrier()
            if stop_after == "hg":
                while WR["next_use"] % len(tile_sched()) != 0:
                    i_, _ = wacq(sched[WR["next_use"]])
                    wrel(i_)
                store_h(t0, False)
                continue

            ffn_ple(0, tl, t0)
            if stop_after == "l0":
                while WR["next_use"] % len(tile_sched()) != 0:
                    i_, _ = wacq(sched[WR["next_use"]])
                    wrel(i_)
                store_h(t0, False)
                continue

            norm_to_xnT()
            with ExitStack() as st:
                cq_b = sb("cq_b", [128, 384], BF16, st=st)
                kv_b = sb("kv_b", [128, 256], BF16, st=st)
                ki_b = sb("ki_b", [128, 128], BF16, st=st)
                cqT = sb("cqT", [128, 3, TT], BF16, st=st)
                widx = sb("widx", [128, NSUB, 8], F32, st=st)
                sq2 = sb("sq2", [128, 2], F32, st=st)
                rq2 = sb("rq2", [128, 2], F32, st=st)
                qnT = sb("qnT", [128, 8, TT], BF16, st=st)
                qiT = sb("qiT", [128, 4, TT], BF16, st=st)
                qlT = sb("qlT", [128, 2, 4, 128], BF16, st=st)
                SC = sb("SC", [128, L], F32, st=st)
                Mk = sb("Mk", [128, L], BF16, st=st)
                MT = sb("MT", [128, NBLK, 128], BF16, st=st)
                rl = [sb(f"rl{i}", [128, 512], F32, st=st) for i in range(2)]
                Lb = [sb("Lb0", [128, 512], F32, st=st)] * 2
                Ee = [sb(f"Ee{i}", [128, 512], BF16, st=st) for i in range(2)]
                Pp = [sb(f"Pp{i}", [128, 512], BF16, st=st) for i in range(2)]
                lo = sb("lo", [128, 1], F32, st=st)
                w0 = sb("w0", [128, 1], F32, st=st)
                Wd = sb("Wd", [128, NIT], F32, st=st)
                thr = sb("thr", [128, 1], F32, st=st)
                cnt_ = sb("cnt_", [128, 1], F32, st=st)
                gw = sb("gw", [128, 1], F32, st=st)
                rden = sb("rden", [128, 4], F32, st=st)
                olat = [sb(f"olat{i}", [128, 256], BF16, st=st) for i in range(2)]
                olT = [sb(f"olT{i}", [128, 2, 128], BF16, st=st) for i in range(2)]
                oTs = sb("oTs", [128, 8, 128], BF16, st=st)

                import os as _os
                DBG = int(_os.environ.get("DSA_DBG", "9"))
                w0i, pan0 = wacq(("at_in", 0))
                w1i, pan1 = wacq(("at_in", 1))
                for s in range(NSUB if DBG >= 0 else 0):
                    blk = tl * NSUB + s
                    p0 = bank()
                    p1 = bank()
                    for kc in range(8):
                        kb.mm(p0[:, 0:456], xnT[:, kc, s * 128:(s + 1) * 128], pan0[:, kc, 0:456],
                              start=(kc == 0), stop=(kc == 7), r=[pan0, xnT], w=[p0])
                    for kc in range(8):
                        kb.mm(p1[:, 0:256], xnT[:, kc, s * 128:(s + 1) * 128], pan1[:, kc, 0:256],
                              start=(kc == 0), stop=(kc == 7), r=[pan1, xnT], w=[p1])
                    SK = _os.environ.get("DSA_SKIP", "").split(",")
                    if "a" not in SK:
                        kb.act(junk[:, 0:384], p0[:, 0:384], AF.Square, r=[p0], w=[junk, sq2], accum=sq2[:, 0:1])
                        kb.act(junk[:, 0:256], p1[:, 0:256], AF.Square, r=[p1], w=[junk, sq2], accum=sq2[:, 1:2])
                        kb.ts(DVE, rq2[:, 0:1], sq2[:, 0:1], 1.0 / 384, EPS, ALU.mult, ALU.add, r=[sq2], w=[rq2])
                        kb.ts(DVE, rq2[:, 1:2], sq2[:, 1:2], 1.0 / 256, EPS, ALU.mult, ALU.add, r=[sq2], w=[rq2])
                        kb.act(rq2[:], rq2[:], AF.Sqrt, r=[rq2], w=[rq2])
                        kb.op(DVE, lambda: nc.vector.reciprocal(out=rq2[:], in_=rq2[:]), r=[rq2], w=[rq2])
                    if "b" not in SK:
                        kb.ts(DVE, cq_b[:], p0[:, 0:384], rq2[:, 0:1], None, ALU.mult, r=[p0, rq2], w=[cq_b])
                        kb.ts(DVE, kv_b[:], p1[:, 0:256], rq2[:, 1:2], None, ALU.mult, r=[p1, rq2], w=[kv_b])
                    if "c" not in SK:
                        kb.copy(ACT, ki_b[:, 0:64], p0[:, 384:448], r=[p0], w=[ki_b])
                        kb.copy(ACT, ki_b[:, 64:128], p0[:, 384:448], r=[p0], w=[ki_b])
                    if "d" not in SK:
                        kb.ts(DVE, widx[:, s, :], p0[:, 448:456], float(8 ** -0.5 * 64 ** -0.5), None, ALU.mult,
                              r=[p0], w=[widx])
                    if "e" not in SK:
                        kb.copy(POOL, ckv1[:, blk, 0:256], kv_b[:], r=[kv_b], w=[ckv1])
                    if "f" in SK:
                        continue
                    tb = tbank()
                    for kc in range(3):
                        kb.tr(tb[:, kc * 128:(kc + 1) * 128], cq_b[:, kc * 128:(kc + 1) * 128], ident[:],
                              r=[cq_b], w=[tb], sig=False)
                    for kc in range(2):
                        kb.tr(tb[:, (3 + kc) * 128:(4 + kc) * 128], kv_b[:, kc * 128:(kc + 1) * 128], ident[:],
                              r=[kv_b], w=[tb], sig=False)
                    kb.tr(tb[:, 5 * 128:6 * 128], ki_b[:], ident[:], r=[ki_b], w=[tb])
                    if "g" in SK:
                        continue
                    kb.copy(ACT, cqT[:, :, s * 128:(s + 1) * 128],
                            tb[:, 0:384].rearrange("p (k t) -> p k t", k=3), r=[tb], w=[cqT])
                    kb.copy(ACT, ckvT[:, :, blk * 128:(blk + 1) * 128],
                            tb[:, 384:640].rearrange("p (k t) -> p k t", k=2), r=[tb], w=[ckvT])
                    kb.copy(ACT, kidxT[:, blk * 128:(blk + 1) * 128], tb[:, 640:768], r=[tb], w=[kidxT])
                wrel(w0i)
                wrel(w1i)
                for half in range(2):
                    wi, pan = wacq(("at_uq", half))
                    for oc in range(4 if DBG >= 1 else 0):
                        ps = bank()
                        for kc in range(3):
                            kb.mm(ps[:], pan[:, kc, oc * 128:(oc + 1) * 128], cqT[:, kc, :], start=(kc == 0),
                                  stop=(kc == 2), r=[pan, cqT], w=[ps])
                        kb.copy(ACT, qnT[:, half * 4 + oc, :], ps[:], r=[ps], w=[qnT])
                    wrel(wi)
                wi, pan = wacq(("at_qi", 0))
                for oc in range(4 if DBG >= 1 else 0):
                    ps = bank()
                    for kc in range(3):
                        kb.mm(ps[:], pan[:, kc, oc * 128:(oc + 1) * 128], cqT[:, kc, :], start=(kc == 0),
                              stop=(kc == 2), r=[pan, cqT], w=[ps])
                    kb.copy(ACT, qiT[:, oc, :], ps[:], r=[ps], w=[qiT])
                wrel(wi)

                import os as _os
                DBG = int(_os.environ.get("DSA_DBG", "9"))
                for qb in range(NSUB if DBG >= 2 else 0):
                    J = tl * NSUB + qb
                    nk = (J + 1) * 128
                    qs_ = slice(qb * 128, (qb + 1) * 128)
                    for kg in range(0, nk, 512):
                        ncol = min(512, nk - kg)
                        for hh in range(8):
                            pr = slice((hh % 2) * 64, (hh % 2) * 64 + 64)
                            ps = bank(PB[4:6])
                            kb.mm(ps[:, 0:ncol], qiT[pr, hh // 2, qs_], kidxT[pr, kg:kg + ncol],
                                  r=[qiT, kidxT], w=[ps])
                            r_ = rl[hh % 2]
                            kb.act(r_[:, 0:ncol], ps[:, 0:ncol], AF.Relu, r=[ps], w=[r_])
                            if hh == 0:
                                kb.ts(DVE, SC[:, kg:kg + ncol], r_[:, 0:ncol], widx[:, qb, 0:1], None, ALU.mult,
                                      r=[r_, widx], w=[SC])
                            else:
                                kb.stt(DVE, SC[:, kg:kg + ncol], r_[:, 0:ncol], widx[:, qb, hh:hh + 1],
                                       SC[:, kg:kg + ncol], ALU.mult, ALU.add, r=[r_, widx, SC], w=[SC])
                    if J >= 2:
                        kb.op(DVE, lambda: nc.vector.tensor_reduce(out=lo[:], in_=SC[:, 0:nk], axis=AX.X, op=ALU.min),
                              r=[SC], w=[lo])
                        kb.op(DVE, lambda: nc.vector.tensor_reduce(out=w0[:], in_=SC[:, 0:nk], axis=AX.X, op=ALU.max),
                              r=[SC], w=[w0])
                        kb.tt(DVE, w0[:], w0[:], lo[:], ALU.subtract, r=[w0, lo], w=[w0])
                        kb.ts(DVE, Wd[:], pow2[:], w0[:, 0:1], None, ALU.mult, r=[pow2, w0], w=[Wd])
                    kb.tt(DVE, SC[:, J * 128:(J + 1) * 128], SC[:, J * 128:(J + 1) * 128], negmask[:], ALU.add,
                          r=[SC, negmask], w=[SC])
                    if J >= 2:
                        for it in range(NIT):
                            kb.tt(DVE, thr[:], lo[:], Wd[:, it:it + 1], ALU.add, r=[lo, Wd], w=[thr])
                            kb.ts(DVE, Mk[:, 0:nk], SC[:, 0:nk], thr[:, 0:1], 0.0, ALU.is_ge, ALU.add,
                                  r=[SC, thr], w=[Mk, cnt_], accum=cnt_[:])
                            kb.stt(DVE, gw[:], cnt_[:], float(TOPK), Wd[:, it:it + 1], ALU.is_ge, ALU.mult,
                                   r=[cnt_, Wd], w=[gw])
                            kb.tt(DVE, lo[:], lo[:], gw[:], ALU.add, r=[lo, gw], w=[lo])
                    else:
                        kb.memset(DVE, lo[:], -1.0e29, w=[lo])
                    kb.ts(DVE, Mk[:, 0:nk], SC[:, 0:nk], lo[:, 0:1], None, ALU.is_ge, r=[SC, lo], w=[Mk])
                    for k4 in range(0, J + 1, 8):
                        n4 = min(8, J + 1 - k4)
                        tb = tbank()
                        for q4 in range(n4):
                            kb.tr(tb[:, q4 * 128:(q4 + 1) * 128], Mk[:, (k4 + q4) * 128:(k4 + q4 + 1) * 128], ident[:],
                                  r=[Mk], w=[tb], sig=(q4 == n4 - 1))
                        kb.copy(POOL if False else ACT, MT[:, k4:k4 + n4, :],
                                tb[:, 0:n4 * 128].rearrange("p (k t) -> p k t", k=n4), r=[tb], w=[MT])
                    jj = 0
                    for hg in range(4 if DBG >= 3 else 0):
                        for h4 in range(4):
                            hh = hg * 4 + h4
                            pr = slice((hh % 2) * 64, (hh % 2) * 64 + 64)
                            ps = bank(PB[4:6])
                            for cc in range(2):
                                kb.mm(ps[:, cc * 128:(cc + 1) * 128], wuk[pr, hh // 2, cc * 128:(cc + 1) * 128],
                                      qnT[pr, hh // 2, qs_], r=[wuk, qnT], w=[ps], sig=(cc == 1))
                            kb.copy(ACT, qlT[:, :, h4, :], ps[:, 0:256].rearrange("p (c t) -> p c t", c=2),
                                    r=[ps], w=[qlT])
                        for kblk in range(J + 1):
                            i2 = jj % 2
                            jj += 1
                            pl = bank(PB[4:6])
                            for cc in range(2):
                                kb.mm(pl[:].rearrange("p (h t) -> p h t", h=4),
                                      ckvT[:, cc, kblk * 128:(kblk + 1) * 128],
                                      qlT[:, cc, :, :], start=(cc == 0), stop=(cc == 1),
                                      r=[ckvT, qlT], w=[pl])
                            dl = J - kblk
                            if dl <= 1:
                                kb.tt(DVE, Lb[i2][:].rearrange("p (h t) -> p h t", h=4),
                                      pl[:].rearrange("p (h t) -> p h t", h=4),
                                      biasT[:, dl, hg * 4:(hg + 1) * 4, :], ALU.add, r=[pl, biasT], w=[Lb[i2]])
                                kb.act(Ee[i2][:], Lb[i2][:], AF.Exp, r=[Lb[i2]], w=[Ee[i2]])
                            else:
                                kb.act(Ee[i2][:], pl[:], AF.Exp, r=[pl], w=[Ee[i2]])
                            kb.tt(POOL if (jj % 3 == 0) else DVE, Pp[i2][:].rearrange("p (h t) -> p h t", h=4),
                                  Ee[i2][:].rearrange("p (h t) -> p h t", h=4),
                                  MT[:, kblk:kblk + 1, :].to_broadcast([128, 4, 128]), ALU.mult,
                                  r=[Ee[i2], MT], w=[Pp[i2]])
                            for h4 in range(4):
                                kb.mm(PB[h4][:, 0:257], Pp[i2][:, h4 * 128:(h4 + 1) * 128], ckv1[:, kblk, 0:257],
                                      start=(kblk == 0), stop=(kblk == J), r=[Pp[i2], ckv1], w=[PB[h4]],
                                      sig=(kblk == J or h4 == 3))
                        for h4 in range(4):
                            hh = hg * 4 + h4
                            i3 = h4 % 2
                            kb.op(DVE, lambda: nc.vector.reciprocal(out=rden[:, h4:h4 + 1], in_=PB[h4][:, 256:257]),
                                  r=[PB[h4]], w=[rden])
                            kb.ts(DVE, olat[i3][:], PB[h4][:, 0:256], rden[:, h4:h4 + 1], None, ALU.mult,
                                  r=[PB[h4], rden], w=[olat[i3]])
                            tb = tbank()
                            for cc in range(2):
                                kb.tr(tb[:, cc * 128:(cc + 1) * 128], olat[i3][:, cc * 128:(cc + 1) * 128], ident[:],
                                      r=[olat[i3]], w=[tb], sig=(cc == 1))
                            kb.copy(ACT, olT[i3][:], tb[:, 0:256].rearrange("p (k t) -> p k t", k=2),
                                    r=[tb], w=[olT[i3]])
                            if h4 % 2 == 0:
                                po = bank(PB[4:6])
                            pr = slice((hh % 2) * 64, (hh % 2) * 64 + 64)
                            for cc in range(2):
                                kb.mm(po[pr, 0:128], wuv[:, cc, hh, :], olT[i3][:, cc, :], start=(cc == 0),
                                      stop=(cc == 1), r=[wuv, olT[i3]], w=[po])
                            if h4 % 2 == 1:
                                kb.copy(ACT, oTs[:, hh // 2, :], po[:, 0:128], r=[po], w=[oTs])
                    for half in range(2):
                        wi, pan = wacq(("at_out", half))
                        if DBG < 4:
                            wrel(wi)
                            continue
                        ps = bank(PB[4:6])
                        for j in range(8):
                            kb.mm(ps[:], oTs[:, j, :], pan[:, j, :], start=(j == 0), stop=(j == 7),
                                  r=[oTs, pan], w=[ps])
                        hs = h[:, qb, half * 512:(half + 1) * 512]
                        kb.tt(DVE, hs, ps[:], hs, ALU.add, r=[ps, h.regs[qb]], w=[h.regs[qb]])
                        wrel(wi)
                kb.barrier()
            if stop_after == "at":
                while WR["next_use"] % len(tile_sched()) != 0:
                    i_, _ = wacq(sched[WR["next_use"]])
                    wrel(i_)
                store_h(t0, False)
                continue

            ffn_ple(1, tl, t0)
            store_h(t0, stop_after != "f1")

        kb.barrier()
    print("instructions emitted:", kb.ninst, "sems:", kb.nsem)
    return nc


_CACHE = {}


def _run(inputs, L, n_cores, stop_after=None):
    key = (L, stop_after)
    if key not in _CACHE:
        _CACHE[key] = build(L, stop_after)
    nc = _CACHE[key]
    consts = host_consts()
    in_maps = []
    for c in range(n_cores):
        m = {"x": np.ascontiguousarray(inputs["x"][c, :L]),
             "p": np.ascontiguousarray(inputs["p"][:, c, :L])}
        for n, _ in IN_SPECS:
            m[n] = np.ascontiguousarray(np.asarray(inputs[n], dtype=np.float32))
        m.update(consts)
        in_maps.append(m)
    import os as _o
    res = run_bass_kernel_spmd(nc, in_maps, core_ids=list(range(n_cores)), trace=bool(_o.environ.get("KTRACE")))
    return np.stack([np.asarray(r["y"]) for r in res.results], axis=0), res


def kernel(**inputs):
    inputs = {k: np.asarray(v) for k, v in inputs.items()}
    out, _ = _run(inputs, 4096, 8)
    return out.astype(np.float32)
```

```python
import math
from contextlib import ExitStack

import numpy as np
import ml_dtypes
import concourse.bass as bass
import concourse.mybir as mybir
from concourse.bass_utils import run_bass_kernel_spmd

F32 = mybir.dt.float32
BF16 = mybir.dt.bfloat16
AF = mybir.ActivationFunctionType
ALU = mybir.AluOpType
AX = mybir.AxisListType

D = 1024
TT = 512
NSUB = 4
DFF = 2816
EPS = 1e-6
NIT = 22
TOPK = 256
NEG = -1.0e30


class Sem:
    def __init__(self, h):
        self.h = h
        self.cnt = 0


class Reg:
    __slots__ = ("name", "w", "rs")

    def __init__(self, name=""):
        self.name = name
        self.w = None
        self.rs = {}


class Buf:
    def __init__(self, t, name, nreg=1):
        self.t = t
        self.reg = Reg(name)
        self.regs = [Reg(f"{name}{i}") for i in range(nreg)] if nreg > 1 else [self.reg]

    def __getitem__(self, k):
        return self.t[k]


def _regs(lst):
    out = []
    for b in lst:
        if isinstance(b, Reg):
            out.append(b)
        elif isinstance(b, Buf):
            out.append(b.reg)
        else:
            raise TypeError(type(b))
    return out


class Eng:
    def __init__(self, e, name, sem, same=True):
        self.e = e
        self.name = name
        self.sem = sem
        self.seen = {}
        self.same = same

    def wait(self, tok):
        if tok is None:
            return
        sem, val = tok
        if sem is self.sem and not self.same:
            return
        if self.seen.get(id(sem), 0) >= val:
            return
        self.e.wait_ge(sem.h, val)
        self.seen[id(sem)] = val


class KB:
    def __init__(self, nc, es):
        self.nc = nc
        self.es = es
        mk = lambda n: Sem(es.enter_context(nc.semaphore(n)))
        self.PE = Eng(nc.tensor, "pe", mk("s_pe"), same=False)
        import os as _os
        same = not bool(_os.environ.get("K_NOSAME"))
        self.ACT = Eng(nc.scalar, "act", mk("s_act"), same=same)
        self.DVE = Eng(nc.vector, "dve", mk("s_dve"), same=same)
        self.POOL = Eng(nc.gpsimd, "pool", mk("s_pool"), same=True)
        self.t_wait = {}
        self.SP = Eng(nc.sync, "sp", mk("s_sp"))
        self.compute = [self.PE, self.ACT, self.DVE, self.POOL]
        self.nsem = 5
        self.ninst = 0

    def dsem(self, name):
        self.nsem += 1
        return Sem(self.es.enter_context(self.nc.semaphore(name)))

    def _pre(self, E, r, w):
        for b in r:
            E.wait(b.w)
        for b in w:
            E.wait(b.w)
            for t in b.rs.values():
                E.wait(t)

    def _post(self, tok, r, w):
        for b in r:
            b.rs[id(tok[0])] = tok
        for b in w:
            b.w = tok
            b.rs = {}

    SEM_LIMIT = 10 ** 9

    def op(self, E, emit, r=(), w=(), sig=True):
        r = _regs(r)
        w = _regs(w)
        if E.sem.cnt >= self.SEM_LIMIT:
            self.nsem += 1
            E.sem = Sem(self.es.enter_context(self.nc.semaphore(f"s_{E.name}_{self.nsem}")))
        self._pre(E, r, w)
        ins = emit()
        self.ninst += 1
        if sig:
            E.sem.cnt += 1
            ins.then_inc(E.sem.h, 1)
            tok = (E.sem, E.sem.cnt)
        else:
            tok = (E.sem, E.sem.cnt + 1)
        self._post(tok, r, w)
        return tok

    def dma(self, out, in_, sem, r=(), w=(), Q=None):
        Q = Q or self.SP
        r = _regs(r)
        w = _regs(w)
        self._pre(Q, r, w)
        sem.cnt += 16
        Q.e.dma_start(out=out, in_=in_).then_inc(sem.h, 16)
        self.ninst += 1
        tok = (sem, sem.cnt)
        self._post(tok, r, w)
        return tok

    def barrier(self):
        for E in self.compute + [self.SP]:
            for Fg in self.compute:
                if Fg is not E:
                    E.wait((Fg.sem, Fg.sem.cnt))

    def mm(self, out, lhsT, rhs, start=True, stop=True, r=(), w=(), sig=None):
        if sig is None:
            sig = stop
        return self.op(self.PE, lambda: self.nc.tensor.matmul(out, lhsT, rhs, start=start, stop=stop),
                       r, w, sig)

    def tr(self, out, in_, ident, r=(), w=(), sig=True):
        return self.op(self.PE, lambda: self.nc.tensor.transpose(out, in_, ident), r, w, sig)

    def act(self, out, in_, func, r=(), w=(), bias=None, scale=None, accum=None):
        kw = {}
        if bias is not None:
            kw["bias"] = bias
        if scale is not None:
            kw["scale"] = scale
        if accum is not None:
            kw["accum_out"] = accum
        return self.op(self.ACT, lambda: self.nc.scalar.activation(out=out, in_=in_, func=func, **kw), r, w)

    def ts(self, E, out, in0, s1, s2, op0, op1=None, r=(), w=(), accum=None):
        kw = {}
        if op1 is not None:
            kw["op1"] = op1
        if accum is not None:
            kw["accum_out"] = accum
        return self.op(E, lambda: E.e.tensor_scalar(out=out, in0=in0, scalar1=s1, scalar2=s2, op0=op0, **kw), r, w)

    def tt(self, E, out, in0, in1, op, r=(), w=()):
        return self.op(E, lambda: E.e.tensor_tensor(out=out, in0=in0, in1=in1, op=op), r, w)

    def stt(self, E, out, in0, scalar, in1, op0, op1, r=(), w=()):
        return self.op(E, lambda: E.e.scalar_tensor_tensor(out=out, in0=in0, scalar=scalar, in1=in1,
                                                           op0=op0, op1=op1), r, w)

    def copy(self, E, out, in_, r=(), w=()):
        if E is self.ACT:
            return self.act(out, in_, AF.Copy, r, w)
        return self.op(E, lambda: E.e.tensor_copy(out=out, in_=in_), r, w)

    def memset(self, E, ap, val, w=()):
        return self.op(E, lambda: E.e.memset(ap, val), (), w)


def rel_bucket_np(n):
    n = np.asarray(n)
    nf = np.maximum(n, 1).astype(np.float32)
    large = 16 + (np.log(nf / np.float32(16)) / np.float32(math.log(8.0)) * np.float32(16)).astype(np.int32)
    large = np.minimum(large, 31)
    return np.where(n < 16, n, large)


def host_consts():
    c = {}
    c["c_ident"] = np.eye(128, dtype=np.float32).astype(ml_dtypes.bfloat16)
    s = np.arange(128)
    c["c_cmaskT"] = (s[:, None] <= s[None, :]).astype(np.float32)
    c["c_negmask"] = np.where(s[None, :] <= s[:, None], 0.0, NEG).astype(np.float32)
    rm = np.ones((128, TT), np.float32)
    rm[:, ::128] = 0.0
    c["c_reset"] = rm
    n = np.arange(-127, 257)
    b = rel_bucket_np(np.maximum(n, 0))
    oh = np.zeros((32, 384), np.float32)
    oh[b, np.arange(384)] = 1.0
    oh[31, :] -= 1.0
    c["c_oh"] = np.ascontiguousarray(oh[:, ::-1])
    c["c_pow2"] = np.tile((0.5 ** np.arange(1, NIT + 1)).astype(np.float32)[None, :], (128, 1))
    return c


CONST_SPECS = [("c_ident", [128, 128], BF16), ("c_cmaskT", [128, 128], F32), ("c_negmask", [128, 128], F32),
               ("c_reset", [128, TT], F32), ("c_oh", [32, 384], F32), ("c_pow2", [128, NIT], F32)]

IN_SPECS = [
    ("hg_norm", [1, 1024]), ("hg_w_in", [1, 1024, 4096]), ("hg_lb", [3, 1024]), ("hg_onorm", [1, 1024]),
    ("hg_w_out", [1, 1024, 1024]), ("at_norm", [1, 1024]), ("at_w_in", [1, 1024, 712]),
    ("at_q_norm", [1, 384]), ("at_kv_norm", [1, 256]), ("at_w_uq", [1, 384, 1024]),
    ("at_w_uk", [1, 16, 64, 256]), ("at_w_uv", [1, 16, 256, 64]), ("at_w_qidx", [1, 384, 512]),
    ("at_w_out", [1, 1024, 1024]), ("rel_bias", [32, 16]), ("ff_norm", [2, 1024]),
    ("ff_w_up", [2, 1024, 5632]), ("ff_conv_w", [2, 3, 5632]), ("ff_conv_b", [2, 5632]),
    ("ff_w_down", [2, 2816, 1024]), ("ple_norm", [2, 1024]), ("ple_w_gate", [2, 1024, 1024]),
    ("ple_w_proj", [2, 256, 1024]), ("final_norm", [1024]),
]


def build(L, stop_after=None):
    NT = L // TT
    NBLK = L // 128
    nc = bass.Bass("TRN2", target_bir_lowering=False)
    dr = {}
    dr["x"] = nc.dram_tensor("x", [L, D], F32, kind="ExternalInput").ap()
    dr["p"] = nc.dram_tensor("p", [2, L, 256], F32, kind="ExternalInput").ap()
    for n, shp in IN_SPECS:
        dr[n] = nc.dram_tensor(n, shp, F32, kind="ExternalInput").ap()
    for n, shp, dt in CONST_SPECS:
        dr[n] = nc.dram_tensor(n, shp, dt, kind="ExternalInput").ap()
    y = nc.dram_tensor("y", [L, D], F32, kind="ExternalOutput").ap()

    WS = {}

    def wspec(name, src, K, gain, panels, scale=1.0):
        KC = K // 128
        NP = max(sum(n for _, n in segs) for segs in panels)
        scr = nc.dram_tensor("scr_" + name, [len(panels), 128, KC, NP], BF16, kind="Internal").ap()
        WS[name] = dict(src=src, KC=KC, gain=gain, panels=panels, scale=scale, scr=scr, NP=NP)

    colp = lambda N, w=512: [[(c, min(w, N - c))] for c in range(0, N, w)]
    wspec("hg_in", dr["hg_w_in"][0], 1024, ("hg_norm", 0), colp(4096))
    wspec("hg_out", dr["hg_w_out"][0], 1024, ("hg_onorm", 0), colp(1024))
    for l in range(2):
        wspec(f"ff_up{l}", dr["ff_w_up"][l], 1024, ("ff_norm", l),
              [[(256 * j, 256), (DFF + 256 * j, 256)] for j in range(11)])
        wspec(f"ff_dn{l}", dr["ff_w_down"][l], 2816, None, colp(1024))
        wspec(f"ple_g{l}", dr["ple_w_gate"][l], 1024, ("ple_norm", l), colp(1024))
        wspec(f"ple_p{l}", dr["ple_w_proj"][l], 256, None, colp(1024))
    wspec("at_in", dr["at_w_in"][0], 1024, ("at_norm", 0), [[(0, 384), (640, 72)], [(384, 256)]])
    wspec("at_uq", dr["at_w_uq"][0], 384, ("at_q_norm", 0), colp(1024))
    wspec("at_qi", dr["at_w_qidx"][0], 384, ("at_q_norm", 0), colp(512))
    wspec("at_out", dr["at_w_out"][0], 1024, None, colp(1024))
    rbv = nc.dram_tensor("scr_rbv", [16, 384], F32, kind="Internal").ap()

    with ExitStack() as es:
        kb = KB(nc, es)
        PE, ACT, DVE, POOL, SP = kb.PE, kb.ACT, kb.DVE, kb.POOL, kb.SP

        uid = {"n": 0}

        def sb(name, shape, dt, nreg=1, st=es):
            uid["n"] += 1
            return Buf(st.enter_context(nc.sbuf_tensor(f"{name}_{uid['n']}", shape, dt)), name, nreg)

        def psb(name, shape, dt):
            return Buf(es.enter_context(nc.psum_tensor(name, shape, dt)), name)

        PB = [psb(f"pb{i}", [128, 512], F32) for i in range(6)]
        PT = [psb(f"pt{i}", [128, 1024], BF16) for i in range(2)]
        rot = {"b": 0, "t": 0}

        def bank(lst=None):
            lst = lst or PB
            rot["b"] += 1
            return lst[rot["b"] % len(lst)]

        def tbank():
            rot["t"] += 1
            return PT[rot["t"] % 2]

        ident = sb("ident", [128, 128], BF16)
        cmaskT = sb("cmaskT", [128, 128], F32)
        negmask = sb("negmask", [128, 128], F32)
        pow2 = sb("pow2", [128, NIT], F32)
        ones_bf = sb("ones_bf", [128, 128], BF16)
        eps_t = sb("eps_t", [128, 1], F32)
        lb = sb("lb", [128, 8], F32)
        oml = sb("oml", [128, 8], F32)
        noml = sb("noml", [128, 8], F32)
        cw = sb("cw", [128, 2, 3, 44], F32)
        cb = sb("cb", [128, 2, 44], F32)
        carry = sb("carry", [128, 2, 44, 2], F32)
        S = sb("S", [128, 8, 128], F32, nreg=8)
        ckvT = sb("ckvT", [128, 2, L], BF16)
        ckv1 = sb("ckv1", [128, NBLK, 258], BF16)
        kidxT = sb("kidxT", [128, L], BF16)
        biasT = sb("biasT", [128, 2, 16, 128], F32)
        wuk = sb("wuk", [128, 8, 256], BF16)
        wuv = sb("wuv", [128, 2, 16, 64], BF16)
        h = sb("h", [128, NSUB, D], F32, nreg=NSUB)
        xnT = sb("xnT", [128, 8, TT], BF16)
        xs = [sb(f"xs{i}", [128, D], BF16) for i in range(2)]
        junk = xs[0]
        gfin = sb("gfin", [128, 1024], F32)
        ss = sb("ss", [128, NSUB], F32)
        rstd = sb("rstd", [128, NSUB], F32)
        NSLOT = 5
        ring_sem = [kb.dsem(f"s_ring{i}") for i in range(NSLOT)]
        s_x = kb.dsem("s_x")
        s_p = kb.dsem("s_p")
        s_y = kb.dsem("s_y")
        s_rm = kb.dsem("s_rm")

        prep_sems = []

        def psem():
            sm = kb.dsem(f"s_prep{len(prep_sems)}")
            prep_sems.append(sm)
            return sm

        def ld(dst_buf, dst_ap, src_ap, sm=None):
            kb.dma(dst_ap, src_ap, sm or psem(), w=[dst_buf])

        ld(ident, ident[:], dr["c_ident"])
        ld(cmaskT, cmaskT[:], dr["c_cmaskT"])
        ld(negmask, negmask[:], dr["c_negmask"])
        ld(pow2, pow2[:], dr["c_pow2"])
        kb.memset(DVE, ones_bf[:], 1.0, w=[ones_bf])
        kb.memset(DVE, eps_t[:], EPS, w=[eps_t])
        kb.memset(DVE, S[:], 0.0, w=S.regs)
        kb.memset(DVE, carry[:], 0.0, w=[carry])
        kb.memset(POOL, ckv1[:], 1.0, w=[ckv1])

        with ExitStack() as st:
            def gain_tile(name, src1d, KC):
                g = sb("g_" + name, [128, KC], F32, st=st)
                with nc.allow_non_contiguous_dma(reason="tiny gain vector load"):
                    ld(g, g[:], src1d.rearrange("(kc p) -> p kc", p=128))
                return g

            gains = {}
            for gname, l, KC in [("hg_norm", 0, 8), ("hg_onorm", 0, 8), ("ff_norm", 0, 8), ("ff_norm", 1, 8),
                                 ("ple_norm", 0, 8), ("ple_norm", 1, 8), ("at_norm", 0, 8), ("at_q_norm", 0, 3),
                                 ("at_kv_norm", 0, 2)]:
                gains[(gname, l)] = gain_tile(f"{gname}{l}", dr[gname][l], KC)
            lbr = sb("lbr", [128, 3, 8], F32, st=st)
            lbe = sb("lbe", [128, 3, 8], F32, st=st)
            lbs = sb("lbs", [128, 8], F32, st=st)
            with nc.allow_non_contiguous_dma(reason="tiny vector load"):
                ld(lbr, lbr[:], dr["hg_lb"].rearrange("s (kc p) -> p s kc", p=128))
                s_cw, s_cb = psem(), psem()
                for l in range(2):
                    ld(cw, cw[:, l], dr["ff_conv_w"][l].rearrange("t (c p) -> p t c", p=128), s_cw)
                    ld(cb, cb[:, l], dr["ff_conv_b"][l].rearrange("(c p) -> p c", p=128), s_cb)
            kb.act(lbe[:], lbr[:], AF.Exp, r=[lbr], w=[lbe])
            kb.tt(DVE, lbs[:], lbe[:, 0, :], lbe[:, 1, :], ALU.add, r=[lbe], w=[lbs])
            kb.tt(DVE, lbs[:], lbs[:], lbe[:, 2, :], ALU.add, r=[lbe, lbs], w=[lbs])
            kb.op(DVE, lambda: nc.vector.reciprocal(out=lbs[:], in_=lbs[:]), r=[lbs], w=[lbs])
            kb.tt(DVE, lb[:], lbe[:, 0, :], lbs[:], ALU.mult, r=[lbe, lbs], w=[lb])
            kb.ts(DVE, oml[:], lb[:], -1.0, 1.0, ALU.mult, ALU.add, r=[lb], w=[oml])
            kb.ts(DVE, noml[:], oml[:], -1.0, None, ALU.mult, r=[oml], w=[noml])

            ld(gfin, gfin[:], dr["final_norm"].partition_broadcast(128))
            stg = [sb(f"stg{i}", [128, 8, 512], F32, st=st) for i in range(2)]
            stb = [sb(f"stb{i}", [128, 8, 512], BF16, st=st) for i in range(2)]
            s_stg = [kb.dsem(f"s_stg{i}") for i in range(2)]
            s_stb = [kb.dsem(f"s_stb{i}") for i in range(2)]
            cnt = {"i": 0, "e": 0}
            cvt_eng = [DVE, ACT, DVE]
            scr_regs = {}
            for name, wsp in WS.items():
                scr_regs[name] = Reg("scr_" + name)
                KC = wsp["KC"]
                src = wsp["src"].rearrange("(kc p) n -> p kc n", p=128)
                g = gains[wsp["gain"]] if wsp["gain"] else None
                for pi, segs in enumerate(wsp["panels"]):
                    npc = sum(n for _, n in segs)
                    for kg in range(0, KC, 8):
                        kcs = min(8, KC - kg)
                        i = cnt["i"] % 2
                        cnt["i"] += 1
                        off = 0
                        for (c0, n) in segs:
                            kb.dma(stg[i][:, 0:kcs, off:off + n], src[:, kg:kg + kcs, c0:c0 + n], s_stg[i],
                                   w=[stg[i]])
                            off += n
                        if g is None:
                            E = cvt_eng[cnt["e"] % 3]
                            cnt["e"] += 1
                            kb.copy(E, stb[i][:, 0:kcs, 0:npc], stg[i][:, 0:kcs, 0:npc], r=[stg[i]], w=[stb[i]])
                        else:
                            for kc in range(kcs):
                                E = cvt_eng[cnt["e"] % 3]
                                cnt["e"] += 1
                                if E is ACT:
                                    kb.act(stb[i][:, kc, 0:npc], stg[i][:, kc, 0:npc], AF.Copy,
                                           r=[stg[i], g], w=[stb[i]], scale=g[:, kg + kc:kg + kc + 1])
                                else:
                                    kb.ts(E, stb[i][:, kc, 0:npc], stg[i][:, kc, 0:npc],
                                          g[:, kg + kc:kg + kc + 1], None, ALU.mult, r=[stg[i], g], w=[stb[i]])
                        kb.dma(wsp["scr"][pi, :, kg:kg + kcs, 0:npc], stb[i][:, 0:kcs, 0:npc], s_stb[i],
                               r=[stb[i]])

            gkv_b = sb("gkv_b", [128, 256], F32, st=st)
            ld(gkv_b, gkv_b[:], dr["at_kv_norm"][0].partition_broadcast(128))
            wuk_f = sb("wuk_f", [128, 8, 256], F32, st=st)
            s_wuk = psem()
            for par in range(2):
                ld(wuk_f, wuk_f[par * 64:(par + 1) * 64, :, :],
                   dr["at_w_uk"][0].rearrange("(j two) d c -> two d j c", two=2)[par], s_wuk)
            for j in range(8):
                kb.stt(DVE, wuk[:, j, :], wuk_f[:, j, :], 0.125, gkv_b[:], ALU.mult, ALU.mult,
                       r=[wuk_f, gkv_b], w=[wuk])
            wuv_f = sb("wuv_f", [128, 2, 16, 64], F32, st=st)
            gkv = gains[("at_kv_norm", 0)]
            s_wuv = psem()
            for cc in range(2):
                ld(wuv_f, wuv_f[:, cc], dr["at_w_uv"][0].rearrange("h (cc c) d -> cc c h d", cc=2)[cc], s_wuv)
            for cc in range(2):
                kb.ts(DVE, wuv[:, cc], wuv_f[:, cc], gkv[:, cc:cc + 1], None, ALU.mult, r=[wuv_f, gkv], w=[wuv])
            rb_sb = sb("rb_sb", [32, 16], F32, st=st)
            oh_sb = sb("oh_sb", [32, 384], F32, st=st)
            rbv_sb = sb("rbv_sb", [16, 384], F32, st=st)
            ld(rb_sb, rb_sb[:], dr["rel_bias"])
            ld(oh_sb, oh_sb[:], dr["c_oh"])
            kb.mm(PB[0][0:16, 0:384], rb_sb[:], oh_sb[:], r=[rb_sb, oh_sb], w=[PB[0]])
            kb.copy(DVE, rbv_sb[:], PB[0][0:16, 0:384], r=[PB[0]], w=[rbv_sb])
            rbv_reg = Reg("rbv")
            kb.dma(rbv, rbv_sb[:], psem(), r=[rbv_sb], w=[rbv_reg])
            s_bias = psem()
            Hk = sb("Hk", [128, 16, 128], F32, st=st)
            for dl in range(2):
                cst = 129 - 128 * dl
                for hh in range(16):
                    src = bass.AP(rbv.tensor, hh * 384 + cst, [[1, 128], [1, 128]])
                    kb.dma(Hk[:, hh, :], src, s_bias, r=[rbv_reg], w=[Hk])
                for hh in range(16):
                    kb.copy(DVE, biasT[:, dl, hh, :], Hk[:, hh, ::-1], r=[Hk], w=[biasT])
            kb.barrier()
            for E in kb.compute + [SP]:
                for sm in prep_sems + s_stb + s_stg:
                    E.wait((sm, sm.cnt))

        ring = [sb(f"ring{i}", [128, 8, 512], BF16) for i in range(NSLOT)]
        def tile_sched():
            sc = []
            sc += [("hg_in", 4), ("hg_in", 5)]
            for gi in range(2):
                sc += [("hg_in", 0 + gi), ("hg_in", 2 + gi), ("hg_in", 6 + gi)]
            sc += [("hg_out", 0), ("hg_out", 1)]

            def ffn(l):
                s2 = [(f"ff_up{l}", j) for j in range(11)]
                for half in range(2):
                    s2 += [(f"ff_dn{l}", half, kg) for kg in range(3)]
                s2 += [(f"ple_g{l}", 0), (f"ple_p{l}", 0), (f"ple_g{l}", 1), (f"ple_p{l}", 1)]
                return s2
            sc += ffn(0)
            sc += [("at_in", 0), ("at_in", 1), ("at_uq", 0), ("at_uq", 1), ("at_qi", 0)]
            sc += [("at_out", 0), ("at_out", 1)] * NSUB
            sc += ffn(1)
            return sc

        sched = tile_sched() * NT
        WR = {"next_load": 0, "next_use": 0, "free": list(range(NSLOT)), "slot_of": {}}

        def _issue_loads():
            while WR["free"] and WR["next_load"] < len(sched):
                i = WR["next_load"]
                it = sched[i]
                name, pi = it[0], it[1]
                wsp = WS[name]
                sl = WR["free"].pop(0)
                KC = wsp["KC"]
                if len(it) == 3:
                    kg = it[2] * 8
                    kcs = min(8, KC - kg)
                else:
                    kg, kcs = 0, KC
                NP = wsp["NP"]
                kb.dma(ring[sl][:, 0:kcs, 0:NP], wsp["scr"][pi, :, kg:kg + kcs, :], ring_sem[sl],
                       w=[ring[sl]])
                WR["slot_of"][i] = sl
                WR["next_load"] += 1

        def wacq(expect):
            i = WR["next_use"]
            assert tuple(sched[i][:len(expect)]) == tuple(expect), (sched[i], expect)
            _issue_loads()
            assert i in WR["slot_of"], "weight ring exhausted"
            WR["next_use"] += 1
            return i, ring[WR["slot_of"][i]]

        def wrel(i):
            WR["free"].append(WR["slot_of"].pop(i))
            _issue_loads()

        def norm_to_xnT():
            for s in range(NSUB):
                kb.act(junk[:], h[:, s, :], AF.Square, r=[h.regs[s]], w=[junk, ss], accum=ss[:, s:s + 1])
            kb.act(rstd[:], ss[:], AF.Sqrt, r=[ss, eps_t], w=[rstd], scale=1.0 / D, bias=eps_t[:])
            kb.op(DVE, lambda: nc.vector.reciprocal(out=rstd[:], in_=rstd[:]), r=[rstd], w=[rstd])
            for s in range(NSUB):
                x_ = xs[s % 2]
                kb.ts(DVE, x_[:], h[:, s, :], rstd[:, s:s + 1], None, ALU.mult, r=[h.regs[s], rstd], w=[x_])
                tb = tbank()
                for kc in range(8):
                    kb.tr(tb[:, kc * 128:(kc + 1) * 128], x_[:, kc * 128:(kc + 1) * 128], ident[:],
                          r=[x_], w=[tb], sig=(kc == 7))
                kb.copy(ACT, xnT[:, :, s * 128:(s + 1) * 128], tb[:].rearrange("p (k t) -> p k t", k=8),
                        r=[tb], w=[xnT])

        def ffn_ple(l, tl, t0):
            norm_to_xnT()
            with ExitStack() as st:
                mT = sb("mT", [128, 22, TT], BF16, st=st)
                U = [sb(f"U{i}", [128, TT + 2], F32, st=st) for i in range(4)]
                cv8 = [sb(f"cv{i}", [128, TT], F32, st=st) for i in range(8)]
                sgt = [sb(f"sgt{i}", [128, TT], F32, st=st) for i in range(2)]
                for j in range(11):
                    cv = cv8[(j % 2) * 4:(j % 2) * 4 + 4]
                    wi, pan = wacq((f"ff_up{l}", j))
                    for blk in range(4):
                        ch = (2 * j + blk) if blk < 2 else (22 + 2 * j + blk - 2)
                        ps = bank()
                        for kc in range(8):
                            kb.mm(ps[:], pan[:, kc, blk * 128:(blk + 1) * 128], xnT[:, kc, :],
                                  start=(kc == 0), stop=(kc == 7), r=[pan, xnT], w=[ps])
                        u = U[blk]
                        c_ = cv[blk]
                        kb.copy(POOL, u[:, 0:2], carry[:, l, ch, :], r=[carry], w=[u])
                        kb.copy(ACT, u[:, 2:TT + 2], ps[:], r=[ps], w=[u])
                        kb.copy(POOL, carry[:, l, ch, :], u[:, TT:TT + 2], r=[u], w=[carry])
                        kb.act(c_[:], ps[:], AF.Identity, r=[ps, cw, cb], w=[c_],
                               scale=cw[:, l, 2, ch:ch + 1], bias=cb[:, l, ch:ch + 1])
                        kb.stt(DVE, c_[:], u[:, 1:TT + 1], cw[:, l, 1, ch:ch + 1], c_[:], ALU.mult, ALU.add,
                               r=[u, cw, c_], w=[c_])
                        kb.stt(DVE, c_[:], u[:, 0:TT], cw[:, l, 0, ch:ch + 1], c_[:], ALU.mult, ALU.add,
                               r=[u, cw, c_], w=[c_])
                    wrel(wi)
                    for q in range(2):
                        sg_ = sgt[q]
                        kb.act(sg_[:], cv[q][:], AF.Silu, r=[cv[q]], w=[sg_])
                        kb.tt(DVE, mT[:, 2 * j + q, :], sg_[:], cv[2 + q][:], ALU.mult, r=[sg_, cv[2 + q]], w=[mT])
                for half in range(2):
                    pans = [wacq((f"ff_dn{l}", half, kg)) for kg in range(3)]
                    for s in range(NSUB):
                        ps = bank()
                        for kc in range(22):
                            pan = pans[kc // 8][1]
                            kb.mm(ps[:], mT[:, kc, s * 128:(s + 1) * 128], pan[:, kc % 8, :],
                                  start=(kc == 0), stop=(kc == 21), r=[pan, mT], w=[ps])
                        hs = h[:, s, half * 512:(half + 1) * 512]
                        kb.tt(DVE, hs, ps[:], hs, ALU.add, r=[ps, h.regs[s]], w=[h.regs[s]])
                    for wi, _ in pans:
                        wrel(wi)
                norm_to_xnT()
                pt = sb("pt", [128, NSUB, 256], F32, st=st)
                ptb = sb("ptb", [128, NSUB, 256], BF16, st=st)
                pT = sb("pT", [128, 2, TT], BF16, st=st)
                sgm = [sb(f"sgm{i}", [128, 512], F32, st=st) for i in range(2)]
                kb.dma(pt[:], dr["p"][l, t0:t0 + TT, :].rearrange("(s p) d -> p s d", p=128), s_p, w=[pt])
                kb.copy(POOL, ptb[:], pt[:], r=[pt], w=[ptb])
                for s in range(NSUB):
                    tb = tbank()
                    for kc in range(2):
                        kb.tr(tb[:, kc * 128:(kc + 1) * 128], ptb[:, s, kc * 128:(kc + 1) * 128], ident[:],
                              r=[ptb], w=[tb], sig=(kc == 1))
                    kb.copy(ACT, pT[:, :, s * 128:(s + 1) * 128],
                            tb[:, 0:256].rearrange("p (k t) -> p k t", k=2), r=[tb], w=[pT])
                for half in range(2):
                    wg, pang = wacq((f"ple_g{l}", half))
                    wp, panp = wacq((f"ple_p{l}", half))
                    for s in range(NSUB):
                        psg = bank()
                        for kc in range(8):
                            kb.mm(psg[:], xnT[:, kc, s * 128:(s + 1) * 128], pang[:, kc, :],
                                  start=(kc == 0), stop=(kc == 7), r=[pang, xnT], w=[psg])
                        psp = bank()
                        for kc in range(2):
                            kb.mm(psp[:], pT[:, kc, s * 128:(s + 1) * 128], panp[:, kc, :],
                                  start=(kc == 0), stop=(kc == 1), r=[panp, pT], w=[psp])
                        sg_ = sgm[s % 2]
                        kb.act(sg_[:], psg[:], AF.Sigmoid, r=[psg], w=[sg_])
                        kb.tt(DVE, sg_[:], sg_[:], psp[:], ALU.mult, r=[sg_, psp], w=[sg_])
                        hs = h[:, s, half * 512:(half + 1) * 512]
                        kb.tt(POOL, hs, hs, sg_[:], ALU.add, r=[sg_, h.regs[s]], w=[h.regs[s]])
                    wrel(wg)
                    wrel(wp)
                kb.barrier()

        def store_h(t0, final):
            with ExitStack() as st:
                ot = sb("ot", [128, NSUB, D], F32, st=st)
                if final:
                    for s in range(NSUB):
                        kb.act(junk[:], h[:, s, :], AF.Square, r=[h.regs[s]], w=[junk, ss], accum=ss[:, s:s + 1])
                    kb.act(rstd[:], ss[:], AF.Sqrt, r=[ss, eps_t], w=[rstd], scale=1.0 / D, bias=eps_t[:])
                    kb.op(DVE, lambda: nc.vector.reciprocal(out=rstd[:], in_=rstd[:]), r=[rstd], w=[rstd])
                    for s in range(NSUB):
                        kb.ts(DVE, ot[:, s, :], h[:, s, :], rstd[:, s:s + 1], None, ALU.mult,
                              r=[h.regs[s], rstd], w=[ot])
                        kb.tt(POOL, ot[:, s, :], ot[:, s, :], gfin[:], ALU.mult, r=[ot, gfin], w=[ot])
                else:
                    for s in range(NSUB):
                        kb.copy(DVE, ot[:, s, :], h[:, s, :], r=[h.regs[s]], w=[ot])
                yreg = Reg("y")
                kb.dma(y[t0:t0 + TT, :].rearrange("(s p) d -> p s d", p=128), ot[:], s_y, r=[ot], w=[yreg])
                for E in kb.compute + [SP]:
                    E.wait((s_y, s_y.cnt))
                kb.barrier()

        for tl in range(NT):
            t0 = tl * TT
            kb.dma(h[:], dr["x"][t0:t0 + TT, :].rearrange("(s p) d -> p s d", p=128), s_x, w=h.regs)

            norm_to_xnT()
            with ExitStack() as st:
                resetm = sb("resetm", [128, TT], F32, st=st)
                kb.dma(resetm[:], dr["c_reset"], s_rm, w=[resetm])
                v_tok = sb("v_tok", [128, NSUB, D], BF16, nreg=NSUB, st=st)
                OG = sb("OG", [128, 8, TT], BF16, st=st)
                qb_ = sb("qb", [128, 4, TT], BF16, nreg=4, st=st)
                kbb = sb("kbb", [128, 4, TT], BF16, nreg=4, st=st)
                gs = sb("gs", [128, 4, TT], F32, nreg=4, st=st)
                tset = [[sb(f"t{k}_{i}", [128, TT], F32, st=st) for k in range(6)] for i in range(2)]
                dlm2 = [sb(f"dlm{i}", [128, 4], F32, st=st) for i in range(2)]
                emid = sb("emid", [128, 4, 4], F32, nreg=4, st=st)
                elast = sb("elast", [128, 4, 4], F32, nreg=4, st=st)
                elm = sb("elm", [128, 4, 4], F32, nreg=4, st=st)
                kbt = [sb(f"kbt{i}", [128, 128], BF16, st=st) for i in range(2)]
                ATm = [sb(f"ATm{i}", [128, 128], BF16, st=st) for i in range(2)]
                Sp = [sb(f"Sp{i}", [128, 128], BF16, st=st) for i in range(2)]
                kvt = [sb(f"kvt{i}", [128, 128], F32, st=st) for i in range(2)]
                osq = sb("osq", [128, TT], BF16, st=st)
                rs_ = sb("rs_", [128, TT], F32, st=st)

                for half in range(2):
                    wi, pan = wacq(("hg_in", 4 + half))
                    for s in range(NSUB):
                        ps = bank()
                        for kc in range(8):
                            kb.mm(ps[:], xnT[:, kc, s * 128:(s + 1) * 128], pan[:, kc, :],
                                  start=(kc == 0), stop=(kc == 7), r=[pan, xnT], w=[ps])
                        kb.copy(ACT, v_tok[:, s, half * 512:(half + 1) * 512], ps[:], r=[ps], w=[v_tok.regs[s]])
                    wrel(wi)
                ii = 0
                for gi in range(2):
                    wq, panq = wacq(("hg_in", 0 + gi))
                    wf, panf = wacq(("hg_in", 2 + gi))
                    wg, pang = wacq(("hg_in", 6 + gi))
                    for hl in range(4):
                        hd = gi * 4 + hl
                        cs_ = slice(hl * 128, (hl + 1) * 128)
                        t_qs, t_sg, t_lf, t_kk, t_cum, t_rel = tset[hl % 2]
                        t_eb, t_enb = t_sg, t_lf
                        dlm = dlm2[hl % 2]

                        def proj(pan):
                            ps = bank()
                            for kc in range(8):
                                kb.mm(ps[:], pan[:, kc, cs_], xnT[:, kc, :], start=(kc == 0), stop=(kc == 7),
                                      r=[pan, xnT], w=[ps])
                            return ps
                        qps = proj(panq)
                        kb.act(t_qs[:], qps[:], AF.Silu, r=[qps], w=[t_qs])
                        gps = proj(pang)
                        kb.act(gs[:, hl, :], gps[:], AF.Silu, r=[gps], w=[gs.regs[hl]])
                        fps = proj(panf)
                        kb.act(t_sg[:], fps[:], AF.Sigmoid, r=[fps], w=[t_sg])
                        kb.act(t_lf[:], t_sg[:], AF.Ln, r=[t_sg, oml, lb], w=[t_lf],
                               scale=oml[:, hd:hd + 1], bias=lb[:, hd:hd + 1])
                        kb.ts(DVE, t_kk[:], t_sg[:], noml[:, hd:hd + 1], oml[:, hd:hd + 1], ALU.mult, ALU.add,
                              r=[t_sg, noml, oml], w=[t_kk])
                        kb.op(DVE, lambda: nc.vector.tensor_tensor_scan(
                            out=t_cum[:], data0=resetm[:], data1=t_lf[:], initial=0.0,
                            op0=ALU.mult, op1=ALU.add), r=[resetm, t_lf], w=[t_cum])
                        cum3 = t_cum[:].rearrange("p (c j) -> p c j", j=128)
                        kb.tt(DVE, t_rel[:].rearrange("p (c j) -> p c j", j=128), cum3,
                              cum3[:, :, 63:64].to_broadcast([128, 4, 128]), ALU.subtract, r=[t_cum], w=[t_rel])
                        kb.act(t_eb[:], t_rel[:], AF.Exp, r=[t_rel], w=[t_eb])
                        kb.act(t_enb[:], t_rel[:], AF.Exp, r=[t_rel], w=[t_enb], scale=-1.0)
                        kb.act(emid[:, hl, :], cum3[:, :, 63], AF.Exp, r=[t_cum], w=[emid.regs[hl]])
                        kb.act(elast[:, hl, :], cum3[:, :, 127], AF.Exp, r=[t_cum], w=[elast.regs[hl]])
                        kb.tt(DVE, dlm[:], cum3[:, :, 127], cum3[:, :, 63], ALU.subtract, r=[t_cum], w=[dlm])
                        kb.act(elm[:, hl, :], dlm[:], AF.Exp, r=[dlm], w=[elm.regs[hl]])
                        kb.tt(DVE, qb_[:, hl, :], t_qs[:], t_eb[:], ALU.mult, r=[t_qs, t_eb], w=[qb_.regs[hl]])
                        kb.tt(POOL, kbb[:, hl, :], t_kk[:], t_enb[:], ALU.mult, r=[t_kk, t_enb], w=[kbb.regs[hl]])
                    wrel(wq)
                    wrel(wf)
                    wrel(wg)
                    for c in range(4):
                        cs = slice(c * 128, (c + 1) * 128)
                        for hl in range(4):
                            hd = gi * 4 + hl
                            i2 = ii % 2
                            ii += 1
                            vh = v_tok[:, c, hd * 128:(hd + 1) * 128]
                            tb = tbank()
                            kb.tr(tb[:, 0:128], kbb[:, hl, cs], ident[:], r=[kbb.regs[hl]], w=[tb])
                            kb.copy(ACT, kbt[i2][:], tb[:, 0:128], r=[tb], w=[kbt[i2]])
                            pa = bank(PB[4:6])
                            kb.mm(pa[:, 0:128], kbb[:, hl, cs], qb_[:, hl, cs], r=[kbb.regs[hl], qb_.regs[hl]], w=[pa])
                            kb.tt(DVE, ATm[i2][:], pa[:, 0:128], cmaskT[:], ALU.mult, r=[pa, cmaskT], w=[ATm[i2]])
                            kb.ts(POOL, Sp[i2][:], S[:, hd, :], emid[:, hl, c:c + 1], None, ALU.mult,
                                  r=[S.regs[hd], emid.regs[hl]], w=[Sp[i2]])
                            kb.mm(PB[hl][:, cs], vh, ATm[i2][:], start=True, stop=False,
                                  r=[v_tok.regs[c], ATm[i2]], w=[PB[hl]], sig=False)
                            kb.mm(PB[hl][:, cs], Sp[i2][:], qb_[:, hl, cs], start=False, stop=True,
                                  r=[Sp[i2], qb_.regs[hl]], w=[PB[hl]])
                            pk = bank(PB[4:6])
                            kb.mm(pk[:, 0:128], kbt[i2][:], vh, r=[kbt[i2], v_tok.regs[c]], w=[pk])
                            kb.act(kvt[i2][:], pk[:, 0:128], AF.Copy, r=[pk, elm.regs[hl]], w=[kvt[i2]],
                                   scale=elm[:, hl, c:c + 1])
                            kb.stt(DVE, S[:, hd, :], S[:, hd, :], elast[:, hl, c:c + 1], kvt[i2][:],
                                   ALU.mult, ALU.add, r=[S.regs[hd], elast.regs[hl], kvt[i2]], w=[S.regs[hd]])
                    for hl in range(4):
                        hd = gi * 4 + hl
                        kb.act(osq[:], PB[hl][:], AF.Square, r=[PB[hl]], w=[osq])
                        pa = bank(PB[4:6])
                        kb.mm(pa[:], ones_bf[:], osq[:], r=[ones_bf, osq], w=[pa])
                        kb.act(rs_[:], pa[:], AF.Sqrt, r=[pa, eps_t], w=[rs_], scale=1.0 / 128, bias=eps_t[:])
                        kb.op(DVE, lambda: nc.vector.reciprocal(out=rs_[:], in_=rs_[:]), r=[rs_], w=[rs_])
                        kb.tt(POOL, rs_[:], rs_[:], gs[:, hl, :], ALU.mult, r=[rs_, gs.regs[hl]], w=[rs_])
                        kb.tt(DVE, OG[:, hd, :], PB[hl][:], rs_[:], ALU.mult, r=[PB[hl], rs_], w=[OG])
                for half in range(2):
                    wi, pan = wacq(("hg_out", half))
                    for s in range(NSUB):
                        ps = bank()
                        for j in range(8):
                            kb.mm(ps[:], OG[:, j, s * 128:(s + 1) * 128], pan[:, j, :], start=(j == 0), stop=(j == 7),
                                  r=[OG, pan], w=[ps])
                        hs = h[:, s, half * 512:(half + 1) * 512]
                        kb.tt(DVE, hs, ps[:], hs, ALU.add, r=[ps, h.regs[s]], w=[h.regs[s]])
                    wrel(wi)
                kb.barrier()
            if stop_after == "hg":
                while WR["next_use"] % len(tile_sched()) != 0:
                    i_, _ = wacq(sched[WR["next_use"]])
                    wrel(i_)
                store_h(t0, False)
                continue

            ffn_ple(0, tl, t0)
            if stop_after == "l0":
                while WR["next_use"] % len(tile_sched()) != 0:
                    i_, _ = wacq(sched[WR["next_use"]])
                    wrel(i_)
                store_h(t0, False)
                continue

            norm_to_xnT()
            with ExitStack() as st:
                widx = sb("widx", [128, NSUB, 8], F32, st=st)
                qnT = sb("qnT", [128, 8, TT], BF16, st=st)
                qiT = sb("qiT", [128, 4, TT], BF16, st=st)
                st2 = ExitStack()
                cq_b = sb("cq_b", [128, 384], BF16, st=st2)
                kv_b = sb("kv_b", [128, 256], BF16, st=st2)
                ki_b = sb("ki_b", [128, 128], BF16, st=st2)
                cqT = sb("cqT", [128, 3, TT], BF16, st=st2)
                sq2 = sb("sq2", [128, 2], F32, st=st2)
                rq2 = sb("rq2", [128, 2], F32, st=st2)
                import os as _os
                DBG = int(_os.environ.get("DSA_DBG", "9"))
                w0i, pan0 = wacq(("at_in", 0))
                w1i, pan1 = wacq(("at_in", 1))
                for s in range(NSUB if DBG >= 0 else 0):
                    blk = tl * NSUB + s
                    p0 = bank()
                    p1 = bank()
                    for kc in range(8):
                        kb.mm(p0[:, 0:456], xnT[:, kc, s * 128:(s + 1) * 128], pan0[:, kc, 0:456],
                              start=(kc == 0), stop=(kc == 7), r=[pan0, xnT], w=[p0])
                    for kc in range(8):
                        kb.mm(p1[:, 0:256], xnT[:, kc, s * 128:(s + 1) * 128], pan1[:, kc, 0:256],
                              start=(kc == 0), stop=(kc == 7), r=[pan1, xnT], w=[p1])
                    SK = _os.environ.get("DSA_SKIP", "").split(",")
                    if "a" not in SK:
                        kb.act(junk[:, 0:384], p0[:, 0:384], AF.Square, r=[p0], w=[junk, sq2], accum=sq2[:, 0:1])
                        kb.act(junk[:, 0:256], p1[:, 0:256], AF.Square, r=[p1], w=[junk, sq2], accum=sq2[:, 1:2])
                        kb.ts(DVE, rq2[:, 0:1], sq2[:, 0:1], 1.0 / 384, EPS, ALU.mult, ALU.add, r=[sq2], w=[rq2])
                        kb.ts(DVE, rq2[:, 1:2], sq2[:, 1:2], 1.0 / 256, EPS, ALU.mult, ALU.add, r=[sq2], w=[rq2])
                        kb.act(rq2[:], rq2[:], AF.Sqrt, r=[rq2], w=[rq2])
                        kb.op(DVE, lambda: nc.vector.reciprocal(out=rq2[:], in_=rq2[:]), r=[rq2], w=[rq2])
                    if "b" not in SK:
                        kb.ts(DVE, cq_b[:], p0[:, 0:384], rq2[:, 0:1], None, ALU.mult, r=[p0, rq2], w=[cq_b])
                        kb.ts(DVE, kv_b[:], p1[:, 0:256], rq2[:, 1:2], None, ALU.mult, r=[p1, rq2], w=[kv_b])
                    if "c" not in SK:
                        kb.copy(ACT, ki_b[:, 0:64], p0[:, 384:448], r=[p0], w=[ki_b])
                        kb.copy(ACT, ki_b[:, 64:128], p0[:, 384:448], r=[p0], w=[ki_b])
                    if "d" not in SK:
                        kb.ts(DVE, widx[:, s, :], p0[:, 448:456], float(8 ** -0.5 * 64 ** -0.5), None, ALU.mult,
                              r=[p0], w=[widx])
                    if "e" not in SK:
                        kb.copy(POOL, ckv1[:, blk, 0:256], kv_b[:], r=[kv_b], w=[ckv1])
                    if "f" in SK:
                        continue
                    tb = tbank()
                    for kc in range(3):
                        kb.tr(tb[:, kc * 128:(kc + 1) * 128], cq_b[:, kc * 128:(kc + 1) * 128], ident[:],
                              r=[cq_b], w=[tb], sig=False)
                    for kc in range(2):
                        kb.tr(tb[:, (3 + kc) * 128:(4 + kc) * 128], kv_b[:, kc * 128:(kc + 1) * 128], ident[:],
                              r=[kv_b], w=[tb], sig=False)
                    kb.tr(tb[:, 5 * 128:6 * 128], ki_b[:], ident[:], r=[ki_b], w=[tb])
                    if "g" in SK:
                        continue
                    kb.copy(ACT, cqT[:, :, s * 128:(s + 1) * 128],
                            tb[:, 0:384].rearrange("p (k t) -> p k t", k=3), r=[tb], w=[cqT])
                    kb.copy(ACT, ckvT[:, :, blk * 128:(blk + 1) * 128],
                            tb[:, 384:640].rearrange("p (k t) -> p k t", k=2), r=[tb], w=[ckvT])
                    kb.copy(ACT, kidxT[:, blk * 128:(blk + 1) * 128], tb[:, 640:768], r=[tb], w=[kidxT])
                wrel(w0i)
                wrel(w1i)
                for half in range(2):
                    wi, pan = wacq(("at_uq", half))
                    for oc in range(4 if DBG >= 1 else 0):
                        ps = bank()
                        for kc in range(3):
                            kb.mm(ps[:], pan[:, kc, oc * 128:(oc + 1) * 128], cqT[:, kc, :], start=(kc == 0),
                                  stop=(kc == 2), r=[pan, cqT], w=[ps])
                        kb.copy(ACT, qnT[:, half * 4 + oc, :], ps[:], r=[ps], w=[qnT])
                    wrel(wi)
                wi, pan = wacq(("at_qi", 0))
                for oc in range(4 if DBG >= 1 else 0):
                    ps = bank()
                    for kc in range(3):
                        kb.mm(ps[:], pan[:, kc, oc * 128:(oc + 1) * 128], cqT[:, kc, :], start=(kc == 0),
                              stop=(kc == 2), r=[pan, cqT], w=[ps])
                    kb.copy(ACT, qiT[:, oc, :], ps[:], r=[ps], w=[qiT])
                wrel(wi)

                kb.barrier()
                st2.close()
                qlT = sb("qlT", [128, 2, 4, 128], BF16, st=st)
                SC = sb("SC", [128, L], F32, st=st)
                Mk = sb("Mk", [128, L], BF16, st=st)
                MT = sb("MT", [128, NBLK, 128], BF16, st=st)
                rl = [sb(f"rl{i}", [128, 512], F32, st=st) for i in range(2)]
                Lb = [sb("Lb0", [128, 512], F32, st=st)] * 2
                Ee = [sb(f"Ee{i}", [128, 512], BF16, st=st) for i in range(2)]
                Pp = [sb(f"Pp{i}", [128, 512], BF16, st=st) for i in range(2)]
                lo = sb("lo", [128, 1], F32, st=st)
                w0 = sb("w0", [128, 1], F32, st=st)
                Wd = sb("Wd", [128, NIT], F32, st=st)
                thr = sb("thr", [128, 1], F32, st=st)
                cnt_ = sb("cnt_", [128, 1], F32, st=st)
                gw = sb("gw", [128, 1], F32, st=st)
                rden = sb("rden", [128, 4], F32, st=st)
                olat4 = sb("olat4", [128, 4, 256], BF16, st=st)
                olT4 = sb("olT4", [128, 8, 128], BF16, st=st)
                oTs = sb("oTs", [128, 8, 128], BF16, st=st)
                import os as _os
                DBG = int(_os.environ.get("DSA_DBG", "9"))
                for qb in range(NSUB if DBG >= 2 else 0):
                    J = tl * NSUB + qb
                    nk = (J + 1) * 128
                    qs_ = slice(qb * 128, (qb + 1) * 128)
                    for kg in range(0, nk, 512):
                        ncol = min(512, nk - kg)
                        for hh in range(8):
                            pr = slice((hh % 2) * 64, (hh % 2) * 64 + 64)
                            ps = bank(PB[4:6])
                            kb.mm(ps[:, 0:ncol], qiT[pr, hh // 2, qs_], kidxT[pr, kg:kg + ncol],
                                  r=[qiT, kidxT], w=[ps])
                            r_ = rl[hh % 2]
                            kb.act(r_[:, 0:ncol], ps[:, 0:ncol], AF.Relu, r=[ps], w=[r_])
                            if hh == 0:
                                kb.ts(DVE, SC[:, kg:kg + ncol], r_[:, 0:ncol], widx[:, qb, 0:1], None, ALU.mult,
                                      r=[r_, widx], w=[SC])
                            else:
                                kb.stt(DVE, SC[:, kg:kg + ncol], r_[:, 0:ncol], widx[:, qb, hh:hh + 1],
                                       SC[:, kg:kg + ncol], ALU.mult, ALU.add, r=[r_, widx, SC], w=[SC])
                    if J >= 2:
                        kb.op(DVE, lambda: nc.vector.tensor_reduce(out=lo[:], in_=SC[:, 0:nk], axis=AX.X, op=ALU.min),
                              r=[SC], w=[lo])
                        kb.op(DVE, lambda: nc.vector.tensor_reduce(out=w0[:], in_=SC[:, 0:nk], axis=AX.X, op=ALU.max),
                              r=[SC], w=[w0])
                        kb.tt(DVE, w0[:], w0[:], lo[:], ALU.subtract, r=[w0, lo], w=[w0])
                        kb.ts(DVE, Wd[:], pow2[:], w0[:, 0:1], None, ALU.mult, r=[pow2, w0], w=[Wd])
                    kb.tt(DVE, SC[:, J * 128:(J + 1) * 128], SC[:, J * 128:(J + 1) * 128], negmask[:], ALU.add,
                          r=[SC, negmask], w=[SC])
                    if J >= 2:
                        for it in range(NIT):
                            kb.tt(DVE, thr[:], lo[:], Wd[:, it:it + 1], ALU.add, r=[lo, Wd], w=[thr])
                            kb.ts(DVE, Mk[:, 0:nk], SC[:, 0:nk], thr[:, 0:1], 0.0, ALU.is_ge, ALU.add,
                                  r=[SC, thr], w=[Mk, cnt_], accum=cnt_[:])
                            kb.stt(DVE, gw[:], cnt_[:], float(TOPK), Wd[:, it:it + 1], ALU.is_ge, ALU.mult,
                                   r=[cnt_, Wd], w=[gw])
                            kb.tt(DVE, lo[:], lo[:], gw[:], ALU.add, r=[lo, gw], w=[lo])
                    else:
                        kb.memset(DVE, lo[:], -1.0e29, w=[lo])
                    kb.ts(DVE, Mk[:, 0:nk], SC[:, 0:nk], lo[:, 0:1], None, ALU.is_ge, r=[SC, lo], w=[Mk])
                    for k4 in range(0, J + 1, 8):
                        n4 = min(8, J + 1 - k4)
                        tb = tbank()
                        for q4 in range(n4):
                            kb.tr(tb[:, q4 * 128:(q4 + 1) * 128], Mk[:, (k4 + q4) * 128:(k4 + q4 + 1) * 128], ident[:],
                                  r=[Mk], w=[tb], sig=(q4 == n4 - 1))
                        kb.copy(POOL if False else ACT, MT[:, k4:k4 + n4, :],
                                tb[:, 0:n4 * 128].rearrange("p (k t) -> p k t", k=n4), r=[tb], w=[MT])
                    jj = 0
                    for hg in range(4 if DBG >= 3 else 0):
                        for h4 in range(4):
                            hh = hg * 4 + h4
                            pr = slice((hh % 2) * 64, (hh % 2) * 64 + 64)
                            ps = bank(PB[4:6])
                            for cc in range(2):
                                kb.mm(ps[:, cc * 128:(cc + 1) * 128], wuk[pr, hh // 2, cc * 128:(cc + 1) * 128],
                                      qnT[pr, hh // 2, qs_], r=[wuk, qnT], w=[ps], sig=(cc == 1))
                            kb.copy(ACT, qlT[:, :, h4, :], ps[:, 0:256].rearrange("p (c t) -> p c t", c=2),
                                    r=[ps], w=[qlT])
                        def emit_L(kblk_):
                            pl_ = bank(PB[4:6])
                            for cc in range(2):
                                kb.mm(pl_[:].rearrange("p (h t) -> p h t", h=4),
                                      ckvT[:, cc, kblk_ * 128:(kblk_ + 1) * 128],
                                      qlT[:, cc, :, :], start=(cc == 0), stop=(cc == 1),
                                      r=[ckvT, qlT], w=[pl_])
                            return pl_
                        pl_next = emit_L(0)
                        for kblk in range(J + 1):
                            i2 = jj % 2
                            jj += 1
                            pl = pl_next
                            if kblk < J:
                                pl_next = emit_L(kblk + 1)
                            dl = J - kblk
                            if dl <= 1:
                                kb.tt(DVE, Lb[i2][:].rearrange("p (h t) -> p h t", h=4),
                                      pl[:].rearrange("p (h t) -> p h t", h=4),
                                      biasT[:, dl, hg * 4:(hg + 1) * 4, :], ALU.add, r=[pl, biasT], w=[Lb[i2]])
                                kb.act(Ee[i2][:], Lb[i2][:], AF.Exp, r=[Lb[i2]], w=[Ee[i2]])
                            else:
                                kb.act(Ee[i2][:], pl[:], AF.Exp, r=[pl], w=[Ee[i2]])
                            kb.tt(POOL if (jj % 3 == 0) else DVE, Pp[i2][:].rearrange("p (h t) -> p h t", h=4),
                                  Ee[i2][:].rearrange("p (h t) -> p h t", h=4),
                                  MT[:, kblk:kblk + 1, :].to_broadcast([128, 4, 128]), ALU.mult,
                                  r=[Ee[i2], MT], w=[Pp[i2]])
                            for h4 in range(4):
                                kb.mm(PB[h4][:, 0:257], Pp[i2][:, h4 * 128:(h4 + 1) * 128], ckv1[:, kblk, 0:257],
                                      start=(kblk == 0), stop=(kblk == J), r=[Pp[i2], ckv1], w=[PB[h4]],
                                      sig=(kblk == J or h4 == 3))
                        for h4 in range(4):
                            kb.op(DVE, lambda: nc.vector.reciprocal(out=rden[:, h4:h4 + 1], in_=PB[h4][:, 256:257]),
                                  r=[PB[h4]], w=[rden])
                            kb.ts(DVE, olat4[:, h4, :], PB[h4][:, 0:256], rden[:, h4:h4 + 1], None, ALU.mult,
                                  r=[PB[h4], rden], w=[olat4])
                        tb = tbank()
                        for q8 in range(8):
                            kb.tr(tb[:, q8 * 128:(q8 + 1) * 128], olat4[:, q8 // 2, (q8 % 2) * 128:(q8 % 2 + 1) * 128],
                                  ident[:], r=[olat4], w=[tb], sig=(q8 == 7))
                        kb.copy(ACT, olT4[:], tb[:].rearrange("p (k t) -> p k t", k=8), r=[tb], w=[olT4])
                        po = bank(PB[4:6])
                        for h4 in range(4):
                            hh = hg * 4 + h4
                            pr = slice((hh % 2) * 64, (hh % 2) * 64 + 64)
                            for cc in range(2):
                                kb.mm(po[pr, (h4 // 2) * 128:(h4 // 2 + 1) * 128], wuv[:, cc, hh, :],
                                      olT4[:, h4 * 2 + cc, :], start=(cc == 0), stop=(cc == 1),
                                      r=[wuv, olT4], w=[po])
                        kb.copy(ACT, oTs[:, hg * 2:hg * 2 + 2, :], po[:, 0:256].rearrange("p (k t) -> p k t", k=2),
                                r=[po], w=[oTs])
                    for half in range(2):
                        wi, pan = wacq(("at_out", half))
                        if DBG < 4:
                            wrel(wi)
                            continue
                        ps = bank(PB[4:6])
                        for j in range(8):
                            kb.mm(ps[:], oTs[:, j, :], pan[:, j, :], start=(j == 0), stop=(j == 7),
                                  r=[oTs, pan], w=[ps])
                        hs = h[:, qb, half * 512:(half + 1) * 512]
                        kb.tt(DVE, hs, ps[:], hs, ALU.add, r=[ps, h.regs[qb]], w=[h.regs[qb]])
                        wrel(wi)
                kb.barrier()
            if stop_after == "at":
                while WR["next_use"] % len(tile_sched()) != 0:
                    i_, _ = wacq(sched[WR["next_use"]])
                    wrel(i_)
                store_h(t0, False)
                continue

            ffn_ple(1, tl, t0)
            store_h(t0, stop_after != "f1")

        kb.barrier()
    print("instructions emitted:", kb.ninst, "sems:", kb.nsem)
    return nc


_CACHE = {}


def _run(inputs, L, n_cores, stop_after=None):
    key = (L, stop_after)
    if key not in _CACHE:
        _CACHE[key] = build(L, stop_after)
    nc = _CACHE[key]
    consts = host_consts()
    in_maps = []
    for c in range(n_cores):
        m = {"x": np.ascontiguousarray(inputs["x"][c, :L]),
             "p": np.ascontiguousarray(inputs["p"][:, c, :L])}
        for n, _ in IN_SPECS:
            m[n] = np.ascontiguousarray(np.asarray(inputs[n], dtype=np.float32))
        m.update(consts)
        in_maps.append(m)
    import os as _o
    res = run_bass_kernel_spmd(nc, in_maps, core_ids=list(range(n_cores)), trace=bool(_o.environ.get("KTRACE")))
    return np.stack([np.asarray(r["y"]) for r in res.results], axis=0), res


def kernel(**inputs):
    inputs = {k: np.asarray(v) for k, v in inputs.items()}
    out, _ = _run(inputs, 4096, 8)
    return out.astype(np.float32)
```
